# Optimizing a Trainium2 kernel written in Bass

```python
import jax, jax.numpy as jnp
from jax import lax
import numpy as np

D_MODEL = 1024
BATCH = 16
SEQ = 256
DEPTH = 4
DEC_BATCH = 2
DEC_SEQ = 1024
PAST_LEN = 256

GRID_W = 64
N_MIXERS = 3
N_CONV_LAYERS = (DEPTH + 2) // 3
N_ATTN_LAYERS = (DEPTH + 1) // 3
N_NA_LAYERS = DEPTH // 3
HEAD_DIM = 64
N_HEADS = D_MODEL // HEAD_DIM
N_KV_HEADS = 4
N_NA_HEADS = D_MODEL // HEAD_DIM
QUERY_BLOCK = 128
ROPE_THETA = 10000.0
ROPE_PAIRS = HEAD_DIM // 4
WIN_R = 8
WIN_C = 16
CONV_WIDTH = 3
N_EXPERTS = 32
TOP_K = 4
D_FF = D_MODEL
SWIGLU_LIMIT = 7.0
SWIGLU_ALPHA = 1.702
DEEPNORM_ALPHA = (2 * DEPTH) ** 0.25
DEEPNORM_BETA = (8 * DEPTH) ** -0.25
LN_EPS = 1e-5
RMS_EPS = 1e-6

kernel_name = 'hybrid_diffusion_conv_gqa_natten_moe_step'


def layer_norm(x, g, b):
    xf = x.astype(jnp.float32)
    mu = jnp.mean(xf, -1, keepdims=True)
    var = jnp.mean(jnp.square(xf - mu), -1, keepdims=True)
    y = (xf - mu) * lax.rsqrt(var + LN_EPS)
    return (y * g.astype(jnp.float32) + b.astype(jnp.float32)).astype(x.dtype)


def rms_norm_heads(x, g):
    xf = x.astype(jnp.float32)
    y = xf * lax.rsqrt(jnp.mean(jnp.square(xf), -1, keepdims=True) + RMS_EPS)
    return (y * g.astype(jnp.float32)).astype(x.dtype)


def adaln_params(cond, w_mod, b_mod):
    mod = jax.nn.silu(cond) @ w_mod + b_mod
    return [m[:, None, :] for m in jnp.split(mod, 6, axis=-1)]


def axial_rope(x):
    n = x.shape[1]
    t = jnp.arange(n, dtype=jnp.int32)
    row = (t // GRID_W).astype(jnp.float32)
    col = (t % GRID_W).astype(jnp.float32)
    inv = ROPE_THETA ** (-jnp.arange(ROPE_PAIRS, dtype=jnp.float32) / ROPE_PAIRS)
    xf = x.astype(jnp.float32)

    def rot(xh, pos):
        ang = pos[:, None] * inv[None, :]
        cos = jnp.cos(ang)[None, :, None, :]
        sin = jnp.sin(ang)[None, :, None, :]
        x1, x2 = xh[..., :ROPE_PAIRS], xh[..., ROPE_PAIRS:]
        return jnp.concatenate([x1 * cos - x2 * sin, x1 * sin + x2 * cos], -1)

    half = HEAD_DIM // 2
    out = jnp.concatenate([rot(xf[..., :half], row), rot(xf[..., half:], col)], -1)
    return out.astype(x.dtype)


def blocked_attention(q, k, v):
    b, s, h, dh = q.shape
    g = k.shape[2]
    rep = h // g
    nb = s // QUERY_BLOCK
    qb = q.reshape(b, nb, QUERY_BLOCK, g, rep, dh).transpose(1, 0, 2, 3, 4, 5)
    scale = dh ** -0.5

    def one_block(qblk):
        sc = jnp.einsum('bqgrd,bkgd->bgrqk', qblk, k).astype(jnp.float32) * scale
        p = jax.nn.softmax(sc, axis=-1).astype(v.dtype)
        return jnp.einsum('bgrqk,bkgd->bqgrd', p, v)

    o = lax.map(one_block, qb)
    return o.transpose(1, 0, 2, 3, 4, 5).reshape(b, s, h * dh)


def short_conv_mixer(h, w_in, conv_w, conv_b, w_out):
    gb, gc, xv = jnp.split(h @ w_in, 3, axis=-1)
    u = gc * xv
    pad = CONV_WIDTH // 2
    u = lax.conv_general_dilated(u, conv_w[:, None, :], window_strides=(1,), padding=((pad, pad),),
                                 dimension_numbers=('NWC', 'WIO', 'NWC'),
                                 feature_group_count=u.shape[-1]) + conv_b
    return (gb * u) @ w_out


def gqa_project(h, w_qkv, q_norm, k_norm):
    b, n, _ = h.shape
    q, k, v = jnp.split(h @ w_qkv, [N_HEADS * HEAD_DIM, (N_HEADS + N_KV_HEADS) * HEAD_DIM], axis=-1)
    q = rms_norm_heads(q.reshape(b, n, N_HEADS, HEAD_DIM), q_norm)
    k = rms_norm_heads(k.reshape(b, n, N_KV_HEADS, HEAD_DIM), k_norm)
    v = v.reshape(b, n, N_KV_HEADS, HEAD_DIM)
    return q, k, v


def na_project(h, w_qkv):
    b, n, _ = h.shape
    q, k, v = jnp.split(h @ w_qkv, 3, axis=-1)
    return (q.reshape(b, n, N_NA_HEADS, HEAD_DIM), k.reshape(b, n, N_NA_HEADS, HEAD_DIM),
            v.reshape(b, n, N_NA_HEADS, HEAD_DIM))


def neighbourhood_attention(q, k, v, k_ctx, v_ctx, rpb):
    b, n, h, dh = q.shape
    rows = n // GRID_W
    wr = min(WIN_R, rows)
    t_ctx = k_ctx.shape[1]
    scale = dh ** -0.5
    cols = jnp.arange(GRID_W, dtype=jnp.int32)
    col_start = jnp.clip(cols - WIN_C // 2, 0, GRID_W - WIN_C)
    key_cols = col_start[:, None] + jnp.arange(WIN_C, dtype=jnp.int32)[None, :]
    col_off = key_cols - cols[:, None] + (WIN_C - 1)

    def one_row(r):
        row_start = jnp.clip(r - wr // 2, 0, rows - wr)
        key_rows = row_start + jnp.arange(wr, dtype=jnp.int32)
        idx = (key_rows[None, :, None] * GRID_W + key_cols[:, None, :]).reshape(GRID_W, wr * WIN_C)
        row_off = key_rows - r + (WIN_R - 1)
        bias = rpb[:, row_off[None, :, None], col_off[:, None, :]].reshape(h, GRID_W, wr * WIN_C)
        q_r = lax.dynamic_slice_in_dim(q, r * GRID_W, GRID_W, axis=1)
        k_g = jnp.take(k, idx, axis=1)
        v_g = jnp.take(v, idx, axis=1)
        s_loc = jnp.einsum('bqhd,bqkhd->bhqk', q_r, k_g).astype(jnp.float32) * scale \
            + bias.astype(jnp.float32)[None]
        s_ctx = jnp.einsum('bqhd,bkhd->bhqk', q_r, k_ctx).astype(jnp.float32) * scale
        p = jax.nn.softmax(jnp.concatenate([s_ctx, s_loc], -1), axis=-1).astype(v.dtype)
        return (jnp.einsum('bhqk,bkhd->bqhd', p[..., :t_ctx], v_ctx)
                + jnp.einsum('bhqk,bqkhd->bqhd', p[..., t_ctx:], v_g))

    o = lax.map(one_row, jnp.arange(rows, dtype=jnp.int32))
    return o.transpose(1, 0, 2, 3, 4).reshape(b, n, h * dh)


def clamped_swiglu(hh):
    gate, up = hh[..., :D_FF], hh[..., D_FF:]
    gate = jnp.minimum(gate, SWIGLU_LIMIT)
    up = jnp.clip(up, -SWIGLU_LIMIT, SWIGLU_LIMIT)
    return gate * jax.nn.sigmoid(SWIGLU_ALPHA * gate) * (up + 1.0)


def moe_ffn(h, router_w, router_b, w1, b1, w2, b2):
    b, n, d = h.shape
    xt = h.reshape(b * n, d)
    logits = (xt @ router_w + router_b).astype(jnp.float32)
    top_vals, top_idx = lax.top_k(logits, TOP_K)
    top_w = jax.nn.softmax(top_vals, axis=-1)
    combine = jnp.einsum('tk,tke->te', top_w,
                         jax.nn.one_hot(top_idx, N_EXPERTS, dtype=jnp.float32)).astype(h.dtype)
    out = jnp.zeros_like(xt)
    for e in range(N_EXPERTS):
        y = clamped_swiglu(xt @ w1[e] + b1[e]) @ w2[e] + b2[e]
        out = out + combine[:, e:e + 1] * y
    return out.reshape(b, n, d)


def setup_inputs(seed: int = 0) -> dict:
    key = jax.random.key(seed)
    ks = iter(jax.random.split(key, 32))

    def nrm(shape, scale):
        return jax.random.normal(next(ks), shape, jnp.float32) * scale

    d = D_MODEL
    qkv_w = (N_HEADS + 2 * N_KV_HEADS) * HEAD_DIM
    return {
        'x_prompt': nrm((BATCH, SEQ, d), 1.0),
        'x_sample': nrm((DEC_BATCH, DEC_SEQ, d), 1.0),
        'cache_k_attn': nrm((DEC_BATCH, N_ATTN_LAYERS, PAST_LEN, N_KV_HEADS, HEAD_DIM), 1.0),
        'cache_v_attn': nrm((DEC_BATCH, N_ATTN_LAYERS, PAST_LEN, N_KV_HEADS, HEAD_DIM), 1.0),
        'cache_k_na': nrm((DEC_BATCH, N_NA_LAYERS, PAST_LEN, N_NA_HEADS, HEAD_DIM), 1.0),
        'cache_v_na': nrm((DEC_BATCH, N_NA_LAYERS, PAST_LEN, N_NA_HEADS, HEAD_DIM), 1.0),
        'c': nrm((DEC_BATCH, d), 1.0),
        'c_ctx': nrm((d,), 1.0),
        'w_mod': nrm((DEPTH, d, 6 * d), 0.5 * d ** -0.5),
        'b_mod': nrm((DEPTH, 6 * d), 0.02),
        'ln1_g': 1.0 + nrm((DEPTH, d), 0.02),
        'ln1_b': nrm((DEPTH, d), 0.02),
        'ln2_g': 1.0 + nrm((DEPTH, d), 0.02),
        'ln2_b': nrm((DEPTH, d), 0.02),
        'conv_w_in': nrm((N_CONV_LAYERS, d, 3 * d), d ** -0.5),
        'conv_w': nrm((N_CONV_LAYERS, CONV_WIDTH, d), CONV_WIDTH ** -0.5),
        'conv_b': nrm((N_CONV_LAYERS, d), 0.02),
        'conv_w_out': nrm((N_CONV_LAYERS, d, d), DEEPNORM_BETA * d ** -0.5),
        'attn_w_qkv': nrm((N_ATTN_LAYERS, d, qkv_w), d ** -0.5),
        'attn_q_norm': 1.0 + nrm((N_ATTN_LAYERS, HEAD_DIM), 0.02),
        'attn_k_norm': 1.0 + nrm((N_ATTN_LAYERS, HEAD_DIM), 0.02),
        'attn_w_o': nrm((N_ATTN_LAYERS, N_HEADS * HEAD_DIM, d), DEEPNORM_BETA * d ** -0.5),
        'na_w_qkv': nrm((N_NA_LAYERS, d, 3 * d), d ** -0.5),
        'na_rpb': nrm((N_NA_LAYERS, N_NA_HEADS, 2 * WIN_R - 1, 2 * WIN_C - 1), 0.05),
        'na_w_o': nrm((N_NA_LAYERS, d, d), DEEPNORM_BETA * d ** -0.5),
        'router_w': nrm((DEPTH, d, N_EXPERTS), d ** -0.5),
        'router_b': nrm((DEPTH, N_EXPERTS), 0.01),
        'moe_w1': nrm((DEPTH, N_EXPERTS, d, 2 * D_FF), d ** -0.5),
        'moe_b1': nrm((DEPTH, N_EXPERTS, 2 * D_FF), 0.02),
        'moe_w2': nrm((DEPTH, N_EXPERTS, D_FF, d), DEEPNORM_BETA * D_FF ** -0.5),
        'moe_b2': nrm((DEPTH, N_EXPERTS, d), 0.02),
    }


def reference(x_prompt, x_sample, cache_k_attn, cache_v_attn, cache_k_na, cache_v_na, c, c_ctx,
              w_mod, b_mod, ln1_g, ln1_b, ln2_g, ln2_b,
              conv_w_in, conv_w, conv_b, conv_w_out,
              attn_w_qkv, attn_q_norm, attn_k_norm, attn_w_o,
              na_w_qkv, na_rpb, na_w_o,
              router_w, router_b, moe_w1, moe_b1, moe_w2, moe_b2):
    xp = x_prompt
    xs = x_sample
    k_attn_list, v_attn_list, k_na_list, v_na_list = [], [], [], []
    for l in range(DEPTH):
        kind, j = l % N_MIXERS, l // N_MIXERS
        sh1p, sc1p, g1p, sh2p, sc2p, g2p = adaln_params(c_ctx[None, :], w_mod[l], b_mod[l])
        sh1s, sc1s, g1s, sh2s, sc2s, g2s = adaln_params(c, w_mod[l], b_mod[l])
        hp = xp * (1 + sc1p) + sh1p
        hs = xs * (1 + sc1s) + sh1s
        if kind == 0:
            op = short_conv_mixer(hp, conv_w_in[j], conv_w[j], conv_b[j], conv_w_out[j])
            os_ = short_conv_mixer(hs, conv_w_in[j], conv_w[j], conv_b[j], conv_w_out[j])
        elif kind == 1:
            qp, kp, vp = gqa_project(hp, attn_w_qkv[j], attn_q_norm[j], attn_k_norm[j])
            k_attn_list.append(kp)
            v_attn_list.append(vp)
            op = blocked_attention(qp, kp, vp) @ attn_w_o[j]
            qs, ks_, vs = gqa_project(hs, attn_w_qkv[j], attn_q_norm[j], attn_k_norm[j])
            qs, ks_ = axial_rope(qs), axial_rope(ks_)
            k_all = jnp.concatenate([cache_k_attn[:, j], ks_], axis=1)
            v_all = jnp.concatenate([cache_v_attn[:, j], vs], axis=1)
            os_ = blocked_attention(qs, k_all, v_all) @ attn_w_o[j]
        else:
            qp, kp, vp = na_project(hp, na_w_qkv[j])
            k_na_list.append(kp)
            v_na_list.append(vp)
            op = blocked_attention(qp, kp, vp) @ na_w_o[j]
            qs, ks_, vs = na_project(hs, na_w_qkv[j])
            os_ = neighbourhood_attention(qs, ks_, vs, cache_k_na[:, j], cache_v_na[:, j],
                                          na_rpb[j]) @ na_w_o[j]
        xp = layer_norm(DEEPNORM_ALPHA * xp + g1p * op, ln1_g[l], ln1_b[l])
        xs = layer_norm(DEEPNORM_ALPHA * xs + g1s * os_, ln1_g[l], ln1_b[l])
        fp = moe_ffn(xp * (1 + sc2p) + sh2p, router_w[l], router_b[l],
                     moe_w1[l], moe_b1[l], moe_w2[l], moe_b2[l])
        fs = moe_ffn(xs * (1 + sc2s) + sh2s, router_w[l], router_b[l],
                     moe_w1[l], moe_b1[l], moe_w2[l], moe_b2[l])
        xp = layer_norm(DEEPNORM_ALPHA * xp + g2p * fp, ln2_g[l], ln2_b[l])
        xs = layer_norm(DEEPNORM_ALPHA * xs + g2s * fs, ln2_g[l], ln2_b[l])
    y_prompt = xp
    y_sample = xs
    new_k_attn = jnp.stack(k_attn_list, axis=1)
    new_v_attn = jnp.stack(v_attn_list, axis=1)
    new_k_na = jnp.stack(k_na_list, axis=1)
    new_v_na = jnp.stack(v_na_list, axis=1)
    return (y_prompt, y_sample, new_k_attn, new_v_attn, new_k_na, new_v_na)
```

```python
import numpy as np
import ml_dtypes
from contextlib import ExitStack
import concourse.bass as bass
import concourse.mybir as mybir
from concourse.bass_utils import run_bass_kernel_spmd

F32 = mybir.dt.float32
BF16 = mybir.dt.bfloat16
ALU = mybir.AluOpType
AF = mybir.ActivationFunctionType

D = 1024
T = 1024
NC_ = 8
NCORES = 6
DEPTH = 4
NE = 32
ALPHA = (2 * DEPTH) ** 0.25
BETA = (8 * DEPTH) ** -0.25
LN_EPS = 1e-5
RMS_EPS = 1e-6
NEG = -30000.0
S7 = float(1.0 / (1.0 + np.exp(-1.702 * 7.0)))
import os
CAP = int(os.environ.get('KCAP', '4000'))
STAGE = int(os.environ.get('KSTAGE', '9'))
LD = int(os.environ.get('KLD', '4'))
NED = int(os.environ.get('KNED', '32'))
SUB = int(os.environ.get('KSUB', '9'))
KDV = int(os.environ.get('KDV', '9'))


class Sched:
    def __init__(self):
        self.ops = []
        self.lastw = {}
        self.rd_eng = {}
        self.rd_dma = {}
        self.seen = set()
        self.seen_order = []
        self.scopes = []
        self.fence = set()
        self.last_eng = {}
        self.last_dma = {}

    def mark(self):
        self.scopes.append(len(self.seen_order))

    def release(self):
        n = self.scopes.pop()
        for x in self.seen_order[n:]:
            self.seen.discard(x)
        del self.seen_order[n:]
        self.fence = set(self.last_eng.values()) | set(self.last_dma.values())

    def add(self, eng, fn, r=(), w=(), dma=None):
        deps = set()
        for x in list(r) + list(w):
            if x not in self.seen:
                self.seen.add(x)
                self.seen_order.append(x)
                deps |= self.fence
        for x in r:
            if x in self.lastw:
                deps.add(self.lastw[x])
        for x in w:
            if x in self.lastw:
                deps.add(self.lastw[x])
            deps.update(self.rd_eng.get(x, {}).values())
            deps.update(self.rd_dma.get(x, ()))
        i = len(self.ops)
        self.ops.append(dict(eng=eng, fn=fn, deps=deps, dma=dma))
        if dma is None:
            self.last_eng[eng] = i
        else:
            self.last_dma[dma] = i
        for x in r:
            if dma is None:
                self.rd_eng.setdefault(x, {})[eng] = i
            else:
                self.rd_dma.setdefault(x, []).append(i)
        for x in w:
            self.lastw[x] = i
            self.rd_eng[x] = {}
            self.rd_dma[x] = []
        return i

    def emit(self, nc, final_waits):
        ops = self.ops
        needed = set()
        for op in ops:
            for d_ in op['deps']:
                if op['eng'] == 'pe' and op['dma'] is None and ops[d_]['eng'] == 'pe' and ops[d_]['dma'] is None:
                    continue
                needed.add(d_)
        for i in final_waits:
            needed.add(i)
        engs = ['pe', 'act', 'dve', 'pool', 'sp']
        seq = {e: 0 for e in engs}
        dcount = {}
        ev = {}
        for i, op in enumerate(ops):
            if op['dma'] is not None:
                n = dcount.get(op['dma'], 0)
                dcount[op['dma']] = n + 1
                ev[i] = (('d', op['dma']), 16 * (n + 1))
            elif i in needed:
                e = op['eng']
                s = seq[e]
                seq[e] = s + 1
                ev[i] = (('e', e, s // CAP), s % CAP + 1)
        semkeys = []
        for i in sorted(ev):
            if ev[i][0] not in semkeys:
                semkeys.append(ev[i][0])
        with ExitStack() as es:
            sems = {}
            for k in semkeys:
                sems[k] = es.enter_context(nc.semaphore("s_" + "_".join(str(x) for x in k[1:]).replace(" ", "")
                                                        .replace("(", "").replace(")", "").replace(",", "_").replace("'", "")))
            block = es.enter_context(nc.Block())
            per = {e: [i for i, op in enumerate(ops) if op['eng'] == e] for e in engs}

            def run(e, eng):
                waited = {}
                for i in per[e]:
                    op = ops[i]
                    for d in sorted(op['deps']):
                        if d not in ev:
                            continue
                        if ops[d]['eng'] == 'pe' and e == 'pe' and ops[d]['dma'] is None:
                            continue
                        k, v = ev[d]
                        if waited.get(k, 0) < v:
                            eng.wait_ge(sems[k], v)
                            waited[k] = v
                    ins = op['fn'](eng)
                    if i in ev:
                        k, v = ev[i]
                        ins.then_inc(sems[k], 16 if k[0] == 'd' else 1)
                if e == 'sp':
                    for i in final_waits:
                        k, v = ev[i]
                        eng.wait_ge(sems[k], v)

            block.tensor(lambda eng: run('pe', eng))
            block.scalar(lambda eng: run('act', eng))
            block.vector(lambda eng: run('dve', eng))
            block.gpsimd(lambda eng: run('pool', eng))
            block.sync(lambda eng: run('sp', eng))


class Arena:
    def __init__(self, ap, words):
        self.ap = ap
        self.words = words
        self.top = 0
        self.marks = []
        self.sched = None

    def alloc(self, words):
        words = (words + 7) // 8 * 8
        o = self.top
        self.top += words
        assert self.top <= self.words, (self.top, self.words)
        return o

    def f32(self, shape):
        n = int(np.prod(shape[1:]))
        o = self.alloc(n)
        v = self.ap[0:shape[0], o:o + n]
        if len(shape) == 3:
            v = v.rearrange("p (a b) -> p a b", a=shape[1])
        elif len(shape) == 4:
            v = v.rearrange("p (a b c) -> p a b c", a=shape[1], b=shape[2])
        return v

    def bf16(self, shape):
        n = int(np.prod(shape[1:]))
        o = self.alloc((n + 1) // 2)
        v = self.ap[0:shape[0], o:o + (n + 1) // 2].bitcast(BF16)[:, 0:n]
        if len(shape) == 3:
            v = v.rearrange("p (a b) -> p a b", a=shape[1])
        elif len(shape) == 4:
            v = v.rearrange("p (a b c) -> p a b c", a=shape[1], b=shape[2])
        return v

    def _shape(self, v, shape):
        if len(shape) == 3:
            v = v.rearrange("p (a b) -> p a b", a=shape[1])
        elif len(shape) == 4:
            v = v.rearrange("p (a b c) -> p a b c", a=shape[1], b=shape[2])
        return v

    def f32_at(self, o, shape):
        n = int(np.prod(shape[1:]))
        return self._shape(self.ap[0:shape[0], o:o + n], shape)

    def bf16_at(self, o, shape):
        n = int(np.prod(shape[1:]))
        return self._shape(self.ap[0:shape[0], o:o + (n + 1) // 2].bitcast(BF16)[:, 0:n], shape)

    def mark(self):
        self.marks.append(self.top)
        if self.sched is not None:
            self.sched.mark()

    def release(self):
        self.top = self.marks.pop()
        if self.sched is not None:
            self.sched.release()


def build_program(layers, debug_out=False):
    nc = bass.Bass("TRN2", target_bir_lowering=False)
    S = Sched()
    L = DEPTH

    def din(name, shape, dt=F32):
        return nc.dram_tensor(name, list(shape), dt, kind="ExternalInput").ap()

    def dout(name, shape, dt=F32):
        return nc.dram_tensor(name, list(shape), dt, kind="ExternalOutput").ap()

    xT_in = din("xT_in", [D, T])
    cond_in = din("cond", [128, NC_])
    w_mod = din("w_mod", [LD, D, 6 * D])
    bmod_in = din("bmod", [128, L, 48])
    lnp_in = din("lnp", [128, L, 4, NC_])
    router_w = din("router_w", [L, D, NE])
    router_b = din("router_b", [L, NE])
    moe_w1 = din("moe_w1", [LD, NED, D, 2 * D])
    moe_w2 = din("moe_w2", [LD, NED, D, D])
    b1_in = din("b1", [128, L, NE, 16])
    moe_b2 = din("moe_b2", [L, NE, D])
    conv_w_in = din("conv_w_in", [2, D, 3 * D])
    conv_w_out = din("conv_w_out", [2, D, D])
    convp_in = din("convp", [128, 2, 4, NC_])
    cmask_in = din("cmask", [128, 2, T])
    ident_f_in = din("ident_f", [128, 128])
    ident_b_in = din("ident_b", [128, 128], BF16)
    onesD_in = din("onesD", [128, 128])
    ones_in = din("ones_f", [128, 128])
    attn_w_qkv = din("attn_w_qkv", [1, D, 1536])
    attn_w_o = din("attn_w_o", [1, D, D])
    qkn_in = din("qkn", [128, 2])
    rope_in = din("rope", [128, 2, T])
    perm_in = din("perm", [128, 128])
    blk_in = din("blk", [128, 128])
    ckT_attn = din("ckT_attn", [128, 4, 256])
    swp_in = din("swp", [128, 128])
    cv_attn = din("cv_attn", [256, 256])
    amask_in = din("amask", [128, 10, T], BF16)
    na_w_qkv = din("na_w_qkv", [1, D, 3 * D])
    na_w_o = din("na_w_o", [1, D, D])
    ckT_na = din("ckT_na", [D, 256])
    cv_na = din("cv_na", [256, D])
    nmask_in = din("nmask", [128, 10, T], BF16)
    cb2_in = din("cb2", [16, 128, 16 * 64], BF16)

    yT_out = dout("yT_out", [D, T])
    kT_attn_out = dout("kT_attn_out", [256, T])
    v_attn_out = dout("v_attn_out", [T, 256])
    kT_na_out = dout("kT_na_out", [D, T])
    v_na_out = dout("v_na_out", [T, D])

    AW = 53100
    es = ExitStack()
    arena_t = es.enter_context(nc.sbuf_tensor("arena", [128, AW], F32))
    A = Arena(arena_t, AW)
    A.sched = S
    PS = [es.enter_context(nc.psum_tensor("ps%d" % i, [128, 512], F32)) for i in range(8)]

    def ps(i):
        return PS[i][:, :]

    def psr(i):
        return ("ps", i)

    final_waits = []

    xT = A.f32([128, NC_, T])
    hb = A.bf16([128, NC_, T])
    R = A.f32([128, NC_, T])
    ident_f = A.f32([128, 128])
    onesD = A.f32([128, 128])
    ones_f = A.f32([128, 128])
    ident_b = A.bf16([128, 128])
    condT = A.f32([128, NC_])
    scT = A.bf16([128, NC_])
    bmod = A.f32([128, L, 48])
    lnp = A.f32([128, L, 4, NC_])
    convp = A.f32([128, 2, 4, NC_])
    modT = A.f32([128, 48])
    SQ = [A.f32([128, 512]) for _ in range(2)]
    RS = A.f32([128, 512])

    def sp_load(dst, src, name, eng='sp', res=None):
        S.add(eng, lambda e, d=dst, s=src: e.dma_start(out=d, in_=s), w=(res or [name]), dma=name)

    sp_load(xT, xT_in.rearrange("(c p) t -> p c t", p=128), "xT", res=[("xT", c) for c in range(NC_)])
    sp_load(condT, cond_in, "condT")
    sp_load(ident_f, ident_f_in, "ident_f")
    sp_load(onesD, onesD_in, "onesD")
    sp_load(ones_f, ones_in, "ones_f")
    sp_load(ident_b, ident_b_in, "ident_b")
    sp_load(bmod, bmod_in, "bmod")
    sp_load(lnp, lnp_in, "lnp")
    sp_load(convp, convp_in, "convp")
    S.add('act', lambda e: e.activation(out=scT, in_=condT, func=AF.Silu), r=["condT"], w=["scT"])

    def adaln(l):
        A.mark()
        Wm = [A.bf16([128, NC_, 1024]) for _ in range(6)]
        for g in range(6):
            wb = Wm[g]
            nm = ("Wm", g)
            S.add('pool', lambda e, wb=wb, g=g: e.dma_start(
                out=wb, in_=w_mod[l, :, g * 1024:(g + 1) * 1024].rearrange("(c p) f -> p c f", p=128)),
                w=[nm], dma=nm)
            for kk in range(8):
                k = g * 8 + kk
                for c in range(NC_):
                    S.add('pe', lambda e, wb=wb, kk=kk, c=c, k=k: e.matmul(
                        ps(7)[:, k:k + 1], lhsT=wb[:, c, kk * 128:(kk + 1) * 128], rhs=scT[:, c:c + 1],
                        start=(c == 0), stop=(c == NC_ - 1)),
                        r=[nm, "scT"], w=[psr(7)])
        S.add('dve', lambda e: e.tensor_tensor(out=modT, in0=ps(7)[:, 0:48], in1=bmod[:, l, :], op=ALU.add),
              r=[psr(7), "bmod"], w=["modT"])
        for lo in (8, 32):
            S.add('dve', lambda e, lo=lo: e.tensor_scalar_add(out=modT[:, lo:lo + 8], in0=modT[:, lo:lo + 8], scalar1=1.0),
                  r=["modT"], w=["modT"])
        for lo in (16, 40):
            S.add('dve', lambda e, lo=lo: e.tensor_scalar_mul(out=modT[:, lo:lo + 8], in0=modT[:, lo:lo + 8],
                                                                scalar1=1.0 / ALPHA), r=["modT"], w=["modT"])
        A.release()

    def modulate(src, off):
        for c in range(NC_):
            S.add('dve', lambda e, c=c: e.tensor_scalar(
                out=hb[:, c, :], in0=src[:, c, :], scalar1=modT[:, off + 8 + c:off + 9 + c],
                scalar2=modT[:, off + c:off + c + 1], op0=ALU.mult, op1=ALU.add),
                r=["modT", ("xT", c)], w=[("hb", c)])

    def layernorm(l, which, mod_off, h32=None):
        gi, bi = (0, 1) if which == 1 else (2, 3)
        eps = LN_EPS / (ALPHA * ALPHA)
        for th in range(2):
            cs = slice(th * 512, (th + 1) * 512)
            pm, pv = 5, 6
            for c in range(NC_):
                S.add('pe', lambda e, c=c, cs=cs: e.matmul(ps(pm), lhsT=onesD, rhs=R[:, c, cs],
                                                           start=(c == 0), stop=(c == NC_ - 1)),
                      r=["onesD", ("R", c, th)], w=[psr(pm)])
            for c in range(NC_):
                S.add('dve', lambda e, c=c, cs=cs: e.tensor_tensor(out=R[:, c, cs], in0=R[:, c, cs], in1=ps(pm),
                                                                   op=ALU.subtract),
                      r=[psr(pm), ("R", c, th)], w=[("R", c, th)])
            for c in range(NC_):
                sq = SQ[c % 2]
                S.add('act', lambda e, c=c, cs=cs, sq=sq: e.activation(out=sq, in_=R[:, c, cs], func=AF.Square),
                      r=[("R", c, th)], w=[("SQ", c % 2)])
                S.add('pe', lambda e, c=c, sq=sq: e.matmul(ps(pv), lhsT=onesD, rhs=sq,
                                                           start=(c == 0), stop=(c == NC_ - 1)),
                      r=["onesD", ("SQ", c % 2)], w=[psr(pv)])
            S.add('dve', lambda e: e.tensor_scalar_add(out=RS, in0=ps(pv), scalar1=eps), r=[psr(pv)], w=["RS"])
            S.add('act', lambda e: e.activation(out=RS, in_=RS, func=AF.Sqrt), r=["RS"], w=["RS"])
            S.add('dve', lambda e: e.reciprocal(out=RS, in_=RS), r=["RS"], w=["RS"])
            for c in range(NC_):
                S.add('dve', lambda e, c=c, cs=cs: e.tensor_tensor(out=R[:, c, cs], in0=R[:, c, cs], in1=RS, op=ALU.mult),
                      r=["RS", ("R", c, th)], w=[("R", c, th)])
                S.add('dve', lambda e, c=c, cs=cs: e.tensor_scalar(
                    out=xT[:, c, cs], in0=R[:, c, cs], scalar1=lnp[:, l, gi, c:c + 1], scalar2=lnp[:, l, bi, c:c + 1],
                    op0=ALU.mult, op1=ALU.add), r=["lnp", ("R", c, th)], w=[("xT", c), ("xTh", c, th)])
                if mod_off is not None:
                    S.add('dve', lambda e, c=c, cs=cs: e.tensor_scalar(
                        out=hb[:, c, cs], in0=xT[:, c, cs], scalar1=modT[:, mod_off + 8 + c:mod_off + 9 + c],
                        scalar2=modT[:, mod_off + c:mod_off + c + 1], op0=ALU.mult, op1=ALU.add),
                        r=["modT", ("xTh", c, th)], w=[("hb", c)])
                    if h32 is not None:
                        S.add('dve', lambda e, c=c, cs=cs: e.tensor_scalar(
                            out=h32[:, c, cs], in0=xT[:, c, cs], scalar1=modT[:, mod_off + 8 + c:mod_off + 9 + c],
                            scalar2=modT[:, mod_off + c:mod_off + c + 1], op0=ALU.mult, op1=ALU.add),
                            r=["modT", ("xTh", c, th)], w=[("h32", c), ("W1", 1)])

    def residual_from_psum(c, th, pbank, gate_off):
        cs = slice(th * 512, (th + 1) * 512)
        S.add('dve', lambda e: e.scalar_tensor_tensor(
            out=R[:, c, cs], in0=ps(pbank), scalar=modT[:, gate_off + c:gate_off + c + 1], in1=xT[:, c, cs],
            op0=ALU.mult, op1=ALU.add), r=[psr(pbank), "modT", ("xT", c)], w=[("R", c, th)])

    def conv_mixer(l, j):
        A.mark()
        Wi = [A.bf16([128, NC_, 1024]) for _ in range(2)]
        Wo = A.bf16([128, NC_, 1024])
        U = A.f32([128, NC_, T])
        cmask = A.f32([128, 2, T])
        V = A.bf16([128, NC_, T])
        TMP = [A.f32([128, 512]) for _ in range(2)]
        sp_load(cmask, cmask_in, "cmask")
        for pi, part in enumerate((1, 2, 0)):
            wb = Wi[pi % 2]
            nm = ("Wi", pi % 2)
            S.add('pool', lambda e, wb=wb, part=part: e.dma_start(
                out=wb, in_=conv_w_in[j, :, part * 1024:(part + 1) * 1024].rearrange("(c p) f -> p c f", p=128)),
                w=[nm], dma=nm)
            if pi == 0:
                S.add('pool', lambda e: e.dma_start(out=Wo, in_=conv_w_out[j].rearrange("(c p) f -> p c f", p=128)),
                      w=["Wo"], dma="Wo")
            for fc in range(NC_):
                for th in range(2):
                    cs = slice(th * 512, (th + 1) * 512)
                    pb = (fc * 2 + th) % 4
                    for c in range(NC_):
                        S.add('pe', lambda e, wb=wb, fc=fc, c=c, cs=cs, pb=pb: e.matmul(
                            ps(pb), lhsT=wb[:, c, fc * 128:(fc + 1) * 128], rhs=hb[:, c, cs],
                            start=(c == 0), stop=(c == NC_ - 1)), r=[nm, ("hb", c)], w=[psr(pb)])
                    if part == 1:
                        S.add('act', lambda e, fc=fc, cs=cs, pb=pb: e.copy(out=U[:, fc, cs], in_=ps(pb)),
                              r=[psr(pb)], w=[("U", fc, th)])
                    elif part == 2:
                        S.add('dve', lambda e, fc=fc, cs=cs, pb=pb: e.tensor_tensor(
                            out=U[:, fc, cs], in0=U[:, fc, cs], in1=ps(pb), op=ALU.mult),
                            r=[psr(pb), ("U", fc, th)], w=[("U", fc, th)])
                    else:
                        tmp = TMP[th]
                        tn = ("TMP", th)
                        lo = th * 512
                        S.add('dve', lambda e, fc=fc, cs=cs, tmp=tmp: e.tensor_scalar(
                            out=tmp, in0=U[:, fc, cs], scalar1=convp[:, j, 1, fc:fc + 1], scalar2=convp[:, j, 3, fc:fc + 1],
                            op0=ALU.mult, op1=ALU.add), r=[("U", fc, 0), ("U", fc, 1), "convp"], w=[tn])
                        a0 = max(lo - 1, 0)
                        n0 = 512 - (1 if th == 0 else 0)
                        d0 = 1 if th == 0 else 0
                        A.mark()
                        S.add('dve', lambda e, fc=fc, a0=a0, n0=n0, lo=lo, d0=d0, th=th: e.tensor_tensor(
                            out=SQ[th][:, d0:d0 + n0], in0=U[:, fc, a0:a0 + n0], in1=cmask[:, 0, lo + d0:lo + d0 + n0],
                            op=ALU.mult), r=[("U", fc, 0), ("U", fc, 1), "cmask"], w=[("SQ", th)])
                        S.add('dve', lambda e, fc=fc, d0=d0, n0=n0, tmp=tmp, th=th: e.scalar_tensor_tensor(
                            out=tmp[:, d0:d0 + n0], in0=SQ[th][:, d0:d0 + n0], scalar=convp[:, j, 0, fc:fc + 1],
                            in1=tmp[:, d0:d0 + n0], op0=ALU.mult, op1=ALU.add), r=[("SQ", th), tn, "convp"], w=[tn])
                        n1 = 512 - (1 if th == 1 else 0)
                        S.add('dve', lambda e, fc=fc, lo=lo, n1=n1, th=th: e.tensor_tensor(
                            out=SQ[th][:, 0:n1], in0=U[:, fc, lo + 1:lo + 1 + n1], in1=cmask[:, 1, lo:lo + n1],
                            op=ALU.mult), r=[("U", fc, 0), ("U", fc, 1), "cmask", tn], w=[("SQ", th)])
                        S.add('dve', lambda e, fc=fc, n1=n1, tmp=tmp, th=th: e.scalar_tensor_tensor(
                            out=tmp[:, 0:n1], in0=SQ[th][:, 0:n1], scalar=convp[:, j, 2, fc:fc + 1],
                            in1=tmp[:, 0:n1], op0=ALU.mult, op1=ALU.add), r=[("SQ", th), tn, "convp"], w=[tn])
                        A.release()
                        S.add('dve', lambda e, fc=fc, cs=cs, tmp=tmp, pb=pb: e.tensor_tensor(
                            out=V[:, fc, cs], in0=tmp, in1=ps(pb), op=ALU.mult), r=[tn, psr(pb)], w=[("V", fc)])
        for fc in range(NC_):
            for th in range(2):
                cs = slice(th * 512, (th + 1) * 512)
                pb = (fc * 2 + th) % 4
                for c in range(NC_):
                    S.add('pe', lambda e, fc=fc, c=c, cs=cs, pb=pb: e.matmul(
                        ps(pb), lhsT=Wo[:, c, fc * 128:(fc + 1) * 128], rhs=V[:, c, cs],
                        start=(c == 0), stop=(c == NC_ - 1)), r=["Wo", ("V", c)], w=[psr(pb)])
                residual_from_psum(fc, th, pb, 16)
        A.release()


    def attention(l, j, is_na):
        nvh = 16 if is_na else 4
        NV = nvh * 64
        ng = 8 if is_na else 4
        wqkv = na_w_qkv if is_na else attn_w_qkv
        wo_d = na_w_o if is_na else attn_w_o
        mask_in = nmask_in if is_na else amask_in
        kT_out = kT_na_out if is_na else kT_attn_out
        v_out = v_na_out if is_na else v_attn_out
        A.mark()
        QT = A.bf16([128, NC_, T])
        KT = A.bf16([128, ng, 1280])
        VA = A.bf16([128, 10, nvh, 65])
        OTK = A.bf16([128, 8, D])
        REC = A.f32([128, 8])
        qk2 = A.f32([128, 2])
        A.mark()
        WP = [A.bf16([128, NC_, 512]) for _ in range(2)]
        KST = [A.f32([128, 512]) for _ in range(2)]
        VST = [A.f32([128, NV]) for _ in range(2)]
        if not is_na:
            ROPE = A.f32([128, 2, T])
            PERM = A.f32([128, 128])
            BLK = A.f32([128, 128])
            SWP = A.f32([128, 128])
            QN = A.f32([128, 512])
            T1 = A.f32([128, 512])
            T2 = A.f32([128, 512])
            sp_load(ROPE, rope_in, "ROPE")
            sp_load(PERM, perm_in, "PERM")
            sp_load(BLK, blk_in, "BLK")
            sp_load(SWP, swp_in, "SWP")
            sp_load(qk2, qkn_in, "qk2")
            S.add('dve', lambda e: e.tensor_scalar_mul(out=qk2[:, 0:1], in0=qk2[:, 0:1], scalar1=0.125),
                  r=["qk2"], w=["qk2"])
        S.add('dve', lambda e: e.memset(VA[:, :, :, 64:65], 1.0), w=[("VA1",)])
        if is_na:
            S.add('pool', lambda e: e.dma_start(out=KT[:, :, 0:256], in_=ckT_na.rearrange("(c p) t -> p c t", p=128)),
                  w=[("KTc",)], dma="KTc")
            for blk_ in range(2):
                S.add('pool', lambda e, blk_=blk_: e.dma_start(
                    out=VA[:, blk_, :, 0:64],
                    in_=cv_na[blk_ * 128:(blk_ + 1) * 128, :].rearrange("p (h d) -> p h d", d=64)),
                    w=[("VAc", blk_)], dma=("VAc", blk_))
        else:
            S.add('pool', lambda e: e.dma_start(out=KT[:, :, 0:256], in_=ckT_attn), w=[("KTc",)], dma="KTc")
            for blk_ in range(2):
                S.add('pool', lambda e, blk_=blk_: e.dma_start(
                    out=VA[:, blk_, :, 0:64],
                    in_=cv_attn[blk_ * 128:(blk_ + 1) * 128, :].rearrange("p (h d) -> p h d", d=64)),
                    w=[("VAc", blk_)], dma=("VAc", blk_))
        npieces = 6 if is_na else 3
        kst_i = 0
        for pi in range(npieces):
            wb = WP[pi % 2]
            nm = ("WP", pi % 2)
            S.add('pool', lambda e, wb=wb, pi=pi: e.dma_start(
                out=wb, in_=wqkv[j, :, pi * 512:(pi + 1) * 512].rearrange("(c p) f -> p c f", p=128)),
                w=[nm], dma=nm)
            if pi < 2:
                fm = [("q", pi * 4 + f, f) for f in range(4)]
                vcols = None
            elif is_na and pi < 4:
                fm = [("k", (pi - 2) * 4 + f, f) for f in range(4)]
                vcols = None
            elif is_na:
                fm = []
                vcols = (0, 512, (pi - 4) * 512)
            else:
                fm = [("k", f, f) for f in range(2)]
                vcols = (256, 256, 0)
            for (kind_, gc, fl) in fm:
                for th in range(2):
                    cs = slice(th * 512, (th + 1) * 512)
                    pb = (fl * 2 + th) % 4
                    for c in range(NC_):
                        S.add('pe', lambda e, wb=wb, fl=fl, c=c, cs=cs, pb=pb: e.matmul(
                            ps(pb), lhsT=wb[:, c, fl * 128:(fl + 1) * 128], rhs=hb[:, c, cs],
                            start=(c == 0), stop=(c == NC_ - 1)), r=[nm, ("hb", c)], w=[psr(pb)])
                    if is_na:
                        if kind_ == "q":
                            S.add('act', lambda e, gc=gc, cs=cs, pb=pb: e.mul(out=QT[:, gc, cs], in_=ps(pb), mul=0.125),
                                  r=[psr(pb)], w=[("QT", gc)])
                        else:
                            kst = KST[kst_i % 2]
                            kn = ("KST", kst_i % 2)
                            kst_i += 1
                            S.add('act', lambda e, kst=kst, pb=pb: e.copy(out=kst, in_=ps(pb)), r=[psr(pb)], w=[kn])
                            S.add('dve', lambda e, kst=kst, gc=gc, th=th: e.tensor_copy(
                                out=KT[:, gc, 256 + th * 512:256 + (th + 1) * 512], in_=kst), r=[kn], w=[("KT", gc)])
                            S.add('sp', lambda e, kst=kst, gc=gc, cs=cs: e.dma_start(
                                out=kT_out[gc * 128:(gc + 1) * 128, cs], in_=kst), r=[kn], w=[("kout", gc, th)], dma=kn)
                    else:
                        x = th
                        sq = SQ[x]
                        S.add('act', lambda e, sq=sq, pb=pb: e.activation(out=sq, in_=ps(pb), func=AF.Square),
                              r=[psr(pb)], w=[("SQ", x)])
                        S.add('pe', lambda e, sq=sq: e.matmul(ps(4), lhsT=BLK, rhs=sq, start=True, stop=True),
                              r=["BLK", ("SQ", x)], w=[psr(4)])
                        S.add('dve', lambda e: e.tensor_scalar_add(out=RS, in0=ps(4), scalar1=RMS_EPS), r=[psr(4)], w=["RS"])
                        S.add('act', lambda e: e.activation(out=RS, in_=RS, func=AF.Sqrt), r=["RS"], w=["RS"])
                        S.add('dve', lambda e: e.reciprocal(out=RS, in_=RS), r=["RS"], w=["RS"])
                        gcol = 0 if kind_ == "q" else 1
                        S.add('dve', lambda e, pb=pb, gcol=gcol: e.scalar_tensor_tensor(
                            out=QN, in0=ps(pb), scalar=qk2[:, gcol:gcol + 1], in1=RS, op0=ALU.mult, op1=ALU.mult),
                            r=[psr(pb), "qk2", "RS"], w=["QN"])
                        S.add('pe', lambda e: e.matmul(ps(5), lhsT=PERM, rhs=QN, start=True, stop=True),
                              r=["PERM", "QN"], w=[psr(5)])
                        S.add('dve', lambda e, cs=cs: e.tensor_tensor(out=T1, in0=QN, in1=ROPE[:, 0, cs], op=ALU.mult),
                              r=["QN", "ROPE"], w=["T1"])
                        S.add('dve', lambda e, cs=cs: e.tensor_tensor(out=T2, in0=ROPE[:, 1, cs], in1=ps(5), op=ALU.mult),
                              r=[psr(5), "ROPE"], w=["T2"])
                        if kind_ == "q":
                            S.add('dve', lambda e, gc=gc, cs=cs: e.tensor_tensor(out=QT[:, gc, cs], in0=T1, in1=T2, op=ALU.add),
                                  r=["T1", "T2"], w=[("QT", gc)])
                        else:
                            kst = KST[kst_i % 2]
                            kn = ("KST", kst_i % 2)
                            kst_i += 1
                            kc0 = 256 + th * 512
                            S.add('dve', lambda e, kst=kst: e.tensor_tensor(out=kst, in0=T1, in1=T2, op=ALU.add),
                                  r=["T1", "T2"], w=[kn])
                            S.add('sp', lambda e, kst=kst, gc=gc, cs=cs: e.dma_start(
                                out=kT_out[gc * 128:(gc + 1) * 128, cs], in_=kst), r=[kn], w=[("kout", gc, th)], dma=kn)
                            S.add('act', lambda e, kst=kst, gc=gc, kc0=kc0: e.copy(
                                out=KT[0:64, 2 * gc, kc0:kc0 + 512], in_=kst[0:64, :]), r=[kn], w=[("KT", 2 * gc, 0, th)])
                            S.add('act', lambda e, kst=kst, gc=gc, kc0=kc0: e.copy(
                                out=KT[64:128, 2 * gc + 1, kc0:kc0 + 512], in_=kst[64:128, :]), r=[kn],
                                w=[("KT", 2 * gc + 1, 1, th)])
                            S.add('pe', lambda e, kst=kst: e.matmul(ps(6), lhsT=SWP, rhs=kst, start=True, stop=True),
                                  r=["SWP", kn], w=[psr(6)])
                            S.add('act', lambda e, gc=gc, kc0=kc0: e.copy(
                                out=KT[64:128, 2 * gc, kc0:kc0 + 512], in_=ps(6)[64:128, :]), r=[psr(6)],
                                w=[("KT", 2 * gc, 1, th)])
                            S.add('act', lambda e, gc=gc, kc0=kc0: e.copy(
                                out=KT[0:64, 2 * gc + 1, kc0:kc0 + 512], in_=ps(6)[0:64, :]), r=[psr(6)],
                                w=[("KT", 2 * gc + 1, 0, th)])
            if vcols is not None:
                c0, ncol, vo = vcols
                for tt in range(8):
                    pb = tt % 2
                    ts_ = slice(tt * 128, (tt + 1) * 128)
                    for c in range(NC_):
                        S.add('pe', lambda e, wb=wb, c=c, ts_=ts_, pb=pb, ncol=ncol, c0=c0: e.matmul(
                            ps(pb)[:, 0:ncol], lhsT=hb[:, c, ts_], rhs=wb[:, c, c0:c0 + ncol],
                            start=(c == 0), stop=(c == NC_ - 1)), r=[nm, ("hb", c)], w=[psr(pb)])
                    vst = VST[tt % 2]
                    vn = ("VST", tt % 2)
                    h0 = vo // 64
                    nh = ncol // 64
                    S.add('act', lambda e, vst=vst, pb=pb, vo=vo, ncol=ncol: e.copy(out=vst[:, vo:vo + ncol], in_=ps(pb)[:, 0:ncol]),
                          r=[psr(pb)], w=[vn])
                    S.add('dve', lambda e, tt=tt, pb=pb, h0=h0, nh=nh, ncol=ncol: e.tensor_copy(
                        out=VA[:, 2 + tt, h0:h0 + nh, 0:64],
                        in_=VST[tt % 2][:, h0 * 64:h0 * 64 + ncol].rearrange("p (h d) -> p h d", d=64)),
                        r=[("VST", tt % 2)], w=[("VA", tt, vo)])
                    S.add('sp', lambda e, vst=vst, ts_=ts_, vo=vo, ncol=ncol: e.dma_start(
                        out=v_out[ts_, vo:vo + ncol], in_=vst[:, vo:vo + ncol]), r=[vn], w=[("vout", tt, vo)], dma=vn)
        A.release()
        A.mark()
        MASK = A.bf16([128, 10, T])
        E = A.bf16([128, 10, T])
        CB2 = [A.bf16([128, 16 * 64]) for _ in range(2)] if is_na else None
        sp_load(MASK, mask_in, "MASK")
        qt_all = [("QT", c) for c in range(NC_)]
        kt_all = ([("KT", c) for c in range(ng)] if is_na else
                  [("KT", g_, hf, th) for g_ in range(4) for hf in range(2) for th in range(2)]) + [("KTc",)]
        va_all = [("VA1",), ("VAc", 0), ("VAc", 1)] + [("VA", tt, vo) for tt in range(8) for vo in (range(0, NV, 512) if is_na else [0])]
        for h in range(16):
            hp = (h % 2) * 64
            qc = h // 2
            if is_na:
                ksl = KT[hp:hp + 64, h // 2, :]
                g = h
                cbn = ("CB2", h % 2)
                S.add('sp', lambda e, h=h: e.dma_start(out=CB2[h % 2], in_=cb2_in[h]), w=[cbn], dma=cbn)
            else:
                ksl = KT[hp:hp + 64, h // 4, :]
                g = h // 4
            for b in range(10):
                banks = (0, 1) if b % 2 == 0 else (2, 3)
                for th in range(2):
                    S.add('pe', lambda e, ksl=ksl, b=b, th=th, hp=hp, qc=qc, banks=banks: e.matmul(
                        ps(banks[th]), lhsT=ksl[:, b * 128:(b + 1) * 128],
                        rhs=QT[hp:hp + 64, qc, th * 512:(th + 1) * 512], start=True, stop=False),
                        r=qt_all + kt_all, w=[psr(banks[th])])
                if is_na and b >= 2:
                    bl = b - 2
                    qlo, qhi = max(0, 2 * bl - 7), min(15, 2 * bl + 8)
                    for th in range(2):
                        qa, qb = max(qlo, 8 * th), min(qhi, 8 * th + 7)
                        if qa > qb:
                            continue
                        ja, jb = qa - 2 * bl + 7, qb - 2 * bl + 7
                        S.add('pe', lambda e, th=th, qa=qa, qb=qb, ja=ja, jb=jb, banks=banks, h=h: e.matmul(
                            ps(banks[th])[:, (qa - 8 * th) * 64:(qb + 1 - 8 * th) * 64], lhsT=ident_b,
                            rhs=CB2[h % 2][:, ja * 64:(jb + 1) * 64], start=False, stop=False),
                            r=[cbn, "ident_b"], w=[psr(banks[th])])
                for th in range(2):
                    S.add('pe', lambda e, b=b, th=th, banks=banks: e.matmul(
                        ps(banks[th]), lhsT=ident_b, rhs=MASK[:, b, th * 512:(th + 1) * 512], start=False, stop=True),
                        r=["MASK", "ident_b"], w=[psr(banks[th])])
                for th in range(2):
                    S.add('act', lambda e, b=b, th=th, banks=banks: e.activation(
                        out=E[:, b, th * 512:(th + 1) * 512], in_=ps(banks[th]), func=AF.Exp),
                        r=[psr(banks[th])], w=[("E", b)])
            ob = 4 + 2 * (h % 2)
            for qt in range(8):
                bank = ob + qt // 4
                off = (qt % 4) * 128
                for b in range(10):
                    S.add('pe', lambda e, qt=qt, b=b, bank=bank, off=off, g=g: e.matmul(
                        ps(bank)[:, off:off + 65], lhsT=E[:, b, qt * 128:(qt + 1) * 128], rhs=VA[:, b, g, :],
                        start=(b == 0), stop=(b == 9)), r=[("E", b)] + va_all, w=[psr(bank)])
            for half in range(2):
                bank = ob + half
                S.add('dve', lambda e, bank=bank, half=half: e.reciprocal(
                    out=REC[:, half * 4:(half + 1) * 4],
                    in_=ps(bank).rearrange("p (q c) -> p q c", c=128)[:, :, 64]), r=[psr(bank)], w=[("REC", half)])
                for q4 in range(4):
                    qt = half * 4 + q4
                    S.add('dve', lambda e, bank=bank, q4=q4, qt=qt, h=h: e.tensor_scalar_mul(
                        out=OTK[:, qt, h * 64:(h + 1) * 64], in0=ps(bank)[:, q4 * 128:q4 * 128 + 64],
                        scalar1=REC[:, qt:qt + 1]), r=[psr(bank), ("REC", half)], w=[("OTK", qt)])
        A.release()
        A.mark()
        Wo = A.bf16([128, NC_, 1024])
        S.add('pool', lambda e: e.dma_start(out=Wo, in_=wo_d[j].rearrange("(c p) f -> p c f", p=128)), w=["Wo"], dma="Wo")
        for fc in range(NC_):
            for qg in range(2):
                pb = (fc * 2 + qg) % 4
                pst = PS[pb][:, 0:256].bitcast(BF16)
                for k in range(4):
                    S.add('pe', lambda e, pst=pst, k=k, qg=qg, fc=fc: e.transpose(
                        out=pst[:, k * 128:(k + 1) * 128], in_=OTK[:, qg * 4 + k, fc * 128:(fc + 1) * 128],
                        identity=ident_b), r=[("OTK", qg * 4 + k), "ident_b"], w=[psr(pb)])
                S.add('act', lambda e, pst=pst, fc=fc, qg=qg: e.copy(out=hb[:, fc, qg * 512:(qg + 1) * 512], in_=pst),
                      r=[psr(pb)], w=[("hb", fc)])
        for fc in range(NC_):
            for th in range(2):
                cs = slice(th * 512, (th + 1) * 512)
                pb = (fc * 2 + th) % 4
                for c in range(NC_):
                    S.add('pe', lambda e, fc=fc, c=c, cs=cs, pb=pb: e.matmul(
                        ps(pb), lhsT=Wo[:, c, fc * 128:(fc + 1) * 128], rhs=hb[:, c, cs],
                        start=(c == 0), stop=(c == NC_ - 1)), r=["Wo", ("hb", c)], w=[psr(pb)])
                residual_from_psum(fc, th, pb, 16)
        A.release()
        A.release()

    def moe(l, last):
        A.mark()
        o_w1 = [A.alloc(NC_ * 2048 // 2) for _ in range(2)]
        W1 = [A.bf16_at(o, [128, NC_, 2048]) for o in o_w1]
        h32 = A.f32_at(o_w1[1], [128, NC_, T])
        W2 = A.bf16([128, NC_, 1024])
        aT = A.bf16([128, NC_, T])
        GS = RS
        US = A.f32([128, 512])
        SIG = SQ
        CB = A.f32([128, T])
        b1 = A.f32([128, NE, 16])
        b1s = A.f32([128, NE, 8])
        rw = A.f32([128, NC_, NE])
        rb = A.f32([1, NE])
        o_cb = A.alloc(T)
        combT = A.f32_at(o_cb, [32, T])
        cm = A.f32([32, T])
        b2 = cm
        lg = A.f32([128, NE])
        mx8 = A.f32([128, 8])
        ex = A.f32([128, NE])
        sm = A.f32([128, 4])
        cst = A.f32([128, 4])
        for ci_, val in enumerate((7.0, 8.0, S7, -6.0)):
            S.add('dve', lambda e, ci_=ci_, val=val: e.memset(cst[:, ci_:ci_ + 1], val), w=[("cst", ci_)])
        cstr = [("cst", q) for q in range(4)]

        sp_load(b1, b1_in[:, l], "b1")
        sp_load(b2, moe_b2[l], "cm")
        sp_load(rw, router_w[l].rearrange("(c p) e -> p c e", p=128), "rw")
        sp_load(rb, router_b[l:l + 1, :], "rb")
        S.add('dve', lambda e: e.tensor_scalar_mul(out=b1s, in0=b1[:, :, 0:8], scalar1=1.702), r=["b1"], w=["b1s"])
        S.add('dve', lambda e: e.tensor_scalar_add(out=b1[:, :, 8:16], in0=b1[:, :, 8:16], scalar1=1.0),
              r=["b1"], w=["b1"])

        layernorm(l, 1, 24, h32=h32)

        for tt in range(8):
            ts_ = slice(tt * 128, (tt + 1) * 128)
            for c in range(NC_):
                S.add('pe', lambda e, c=c, ts_=ts_: e.matmul(ps(4)[:, 0:NE], lhsT=h32[:, c, ts_], rhs=rw[:, c, :],
                                                              start=(c == 0), stop=False),
                      r=[("h32", c), "rw"], w=[psr(4)])
            S.add('pe', lambda e: e.matmul(ps(4)[:, 0:NE], lhsT=ones_f[0:1, 0:128], rhs=rb, start=False, stop=True),
                  r=["ones_f", "rb"], w=[psr(4)])
            S.add('dve', lambda e: e.tensor_copy(out=lg, in_=ps(4)[:, 0:NE]), r=[psr(4)], w=["lg"])
            S.add('dve', lambda e: e.max(out=mx8, in_=lg), r=["lg"], w=["mx8"])
            S.add('dve', lambda e: e.tensor_scalar_mul(out=sm[:, 0:1], in0=mx8[:, 0:1], scalar1=-1.0),
                  r=["mx8"], w=["sm0"])
            S.add('act', lambda e: e.activation(out=ex, in_=lg, func=AF.Exp, bias=sm[:, 0:1], scale=1.0),
                  r=["lg", "sm0"], w=["ex"])
            S.add('dve', lambda e: e.scalar_tensor_tensor(out=ex, in0=lg, scalar=mx8[:, 3:4], in1=ex,
                                                          op0=ALU.is_ge, op1=ALU.mult),
                  r=["lg", "mx8", "ex"], w=["ex"])
            S.add('dve', lambda e: e.reduce_sum(out=sm[:, 1:2], in_=ex, axis=mybir.AxisListType.X),
                  r=["ex"], w=["sm1"])
            S.add('dve', lambda e: e.reciprocal(out=sm[:, 2:3], in_=sm[:, 1:2]), r=["sm1"], w=["sm2"])
            S.add('dve', lambda e: e.tensor_scalar_mul(out=ex, in0=ex, scalar1=sm[:, 2:3]), r=["ex", "sm2"], w=["ex"])
            S.add('pe', lambda e: e.transpose(out=ps(4)[0:32, 128:256], in_=ex, identity=ident_f),
                  r=["ex", "ident_f"], w=[psr(4)])
            S.add('dve', lambda e, ts_=ts_: e.tensor_copy(out=combT[:, ts_], in_=ps(4)[0:32, 128:256]),
                  r=[psr(4)], w=["combT"])

        for c in range(NC_):
            for th in range(2):
                cs = slice(th * 512, (th + 1) * 512)
                pb = 4 + (c * 2 + th) % 2
                S.add('pe', lambda e, c=c, cs=cs, pb=pb: e.matmul(ps(pb), lhsT=b2[:, c * 128:(c + 1) * 128],
                                                                   rhs=combT[:, cs], start=True, stop=True),
                      r=["cm", "combT"], w=[psr(pb)])
                S.add('act', lambda e, c=c, cs=cs, pb=pb: e.copy(out=R[:, c, cs], in_=ps(pb)),
                      r=[psr(pb)], w=[("R", c, th)])

        def load_w1(e_):
            nm = ("W1", e_ % 2)
            S.add('pool', lambda e: e.dma_start(out=W1[e_ % 2],
                                                in_=moe_w1[l, e_].rearrange("(c p) f -> p c f", p=128)),
                  w=[nm] + ([("h32", c) for c in range(NC_)] if e_ % 2 == 1 else []), dma=nm)

        def load_w2(e_):
            S.add('pool', lambda e: e.dma_start(out=W2, in_=moe_w2[l, e_].rearrange("(c p) f -> p c f", p=128)),
                  w=["W2"], dma="W2")

        def cb_for(e_):
            S.add('dve', lambda e: e.tensor_scalar_mul(out=cm, in0=combT, scalar1=ident_f[0:32, e_:e_ + 1]),
                  r=["combT", "ident_f"], w=["cm"])
            for th in range(2):
                cs = slice(th * 512, (th + 1) * 512)
                S.add('pe', lambda e, cs=cs, th=th: e.matmul(ps(6 + th), lhsT=ones_f[0:32, 0:128], rhs=cm[:, cs],
                                                             start=True, stop=True),
                      r=["ones_f", "cm"], w=[psr(6 + th)])
                S.add('act', lambda e, cs=cs, th=th: e.copy(out=CB[:, cs], in_=ps(6 + th)),
                      r=[psr(6 + th)], w=[("CB", th)])

        def h1_step(e_, i, th):
            if SUB < 2:
                return
            cs = slice(th * 512, (th + 1) * 512)
            w1 = W1[e_ % 2]
            nm = ("W1", e_ % 2)
            sb = (i + th) % 2
            pg, pu = 2 * sb, 2 * sb + 1
            for c in range(NC_):
                S.add('pe', lambda e, c=c: e.matmul(ps(pg), lhsT=w1[:, c, i * 128:(i + 1) * 128], rhs=hb[:, c, cs],
                                                    start=(c == 0), stop=(c == NC_ - 1)),
                      r=[nm, ("hb", c)], w=[psr(pg)])
            for c in range(NC_):
                S.add('pe', lambda e, c=c: e.matmul(ps(pu), lhsT=w1[:, c, 1024 + i * 128:1024 + (i + 1) * 128],
                                                    rhs=hb[:, c, cs], start=(c == 0), stop=(c == NC_ - 1)),
                      r=[nm, ("hb", c)], w=[psr(pu)])
            sg = SIG[sb]
            if SUB < 3:
                return
            S.add('act', lambda e: e.activation(out=GS, in_=ps(pg), func=AF.Identity, bias=b1[:, e_, i:i + 1], scale=1.0),
                  r=[psr(pg), "b1"], w=["RS"])
            S.add('act', lambda e: e.activation(out=US, in_=ps(pu), func=AF.Identity, bias=b1[:, e_, 8 + i:9 + i], scale=1.0),
                  r=[psr(pu), "b1"], w=["US"])
            if SUB < 4:
                return
            S.add('dve', lambda e: e.tensor_scalar_min(out=GS, in0=GS, scalar1=7.0), r=["RS"], w=["RS"])
            S.add('act', lambda e: e.activation(out=sg, in_=GS, func=AF.Sigmoid, scale=1.702), r=["RS"], w=[("SQ", sb)])
            S.add('dve', lambda e: e.tensor_scalar(out=US, in0=US, scalar1=8.0, scalar2=-6.0, op0=ALU.min, op1=ALU.max),
                  r=["US"], w=["US"])
            S.add('dve', lambda e: e.tensor_tensor(out=GS, in0=GS, in1=sg, op=ALU.mult), r=["RS", ("SQ", sb)], w=["RS"])
            S.add('dve', lambda e: e.tensor_tensor(out=US, in0=US, in1=GS, op=ALU.mult), r=["US", "RS"], w=["US"])
            S.add('dve', lambda e: e.tensor_tensor(out=aT[:, i, cs], in0=US, in1=CB[:, cs], op=ALU.mult),
                  r=["US", ("CB", th)], w=[("aT", i, th)])

        def y_step(e_, c, th):
            if SUB < 5:
                return
            cs = slice(th * 512, (th + 1) * 512)
            pb = 4 + (c + th) % 2
            for k in range(NC_):
                S.add('pe', lambda e, k=k: e.matmul(ps(pb), lhsT=W2[:, k, c * 128:(c + 1) * 128], rhs=aT[:, k, cs],
                                                    start=(k == 0), stop=(k == NC_ - 1)),
                      r=["W2", ("aT", k, th)], w=[psr(pb)])
            S.add('dve', lambda e: e.tensor_tensor(out=R[:, c, cs], in0=R[:, c, cs], in1=ps(pb), op=ALU.add),
                  r=[psr(pb), ("R", c, th)], w=[("R", c, th)])

        NEX = min(NE, NED) if STAGE >= 4 else 0
        if NEX > 0:
            load_w1(0)
        if NEX > 1:
            load_w1(1)
        for e_ in range(NEX):
            load_w2(e_)
            cb_for(e_)
            for i in range(NC_):
                h1_step(e_, i, 0)
            for i in range(NC_):
                h1_step(e_, i, 1)
                y_step(e_, i, 0)
            if e_ + 2 < NEX:
                load_w1(e_ + 2)
            for c in range(NC_):
                y_step(e_, c, 1)

        for c in range(NC_):
            for th in range(2):
                cs = slice(th * 512, (th + 1) * 512)
                S.add('dve', lambda e, c=c, cs=cs: e.scalar_tensor_tensor(
                    out=R[:, c, cs], in0=R[:, c, cs], scalar=modT[:, 40 + c:41 + c], in1=xT[:, c, cs],
                    op0=ALU.mult, op1=ALU.add), r=["modT", ("xT", c), ("R", c, th)], w=[("R", c, th)])
        A.release()

    for li, (l, kind, j) in enumerate(layers):
        if STAGE >= 1:
            adaln(l)
            modulate(xT, 0)
        if STAGE >= 2:
            if kind == 0:
                conv_mixer(l, j)
            elif kind == 1:
                attention(l, j, False)
            else:
                attention(l, j, True)
        if STAGE >= 3:
            moe(l, li == len(layers) - 1)
        if STAGE >= 5:
            layernorm(l, 2, None)

    for c in range(NC_):
        nm = ("yst", c)
        i = S.add('sp', lambda e, c=c: e.dma_start(out=yT_out[c * 128:(c + 1) * 128, :], in_=xT[:, c, :]),
                  r=[("xT", c)] + [("xTh", c, th) for th in range(2)], w=[nm], dma=nm)
        final_waits.append(i)

    S.emit(nc, final_waits)
    es.close()
    return nc


def _fm(v):
    v = np.asarray(v, np.float32)
    lead = v.shape[:-1]
    return np.ascontiguousarray(np.moveaxis(v.reshape(lead + (NC_, 128)), -1, 0))


def _prep_common(inp):
    L = DEPTH
    com = {}
    com["w_mod"] = np.ascontiguousarray(inp["w_mod"][:LD], np.float32)
    com["bmod"] = np.ascontiguousarray(
        np.asarray(inp["b_mod"], np.float32).reshape(L, 48, 128).transpose(2, 0, 1))
    lnp = np.stack([inp["ln1_g"], inp["ln1_b"], inp["ln2_g"], inp["ln2_b"]], axis=1)
    com["lnp"] = np.ascontiguousarray(np.asarray(lnp, np.float32).reshape(L, 4, NC_, 128).transpose(3, 0, 1, 2))
    com["router_w"] = np.ascontiguousarray(inp["router_w"], np.float32)
    com["router_b"] = np.ascontiguousarray(inp["router_b"], np.float32)
    com["moe_w1"] = np.ascontiguousarray(inp["moe_w1"][:LD, :NED], np.float32)
    com["moe_w2"] = np.ascontiguousarray(inp["moe_w2"][:LD, :NED], np.float32)
    com["b1"] = np.ascontiguousarray(
        np.asarray(inp["moe_b1"], np.float32).reshape(L, NE, 16, 128).transpose(3, 0, 1, 2))
    com["moe_b2"] = np.ascontiguousarray(inp["moe_b2"], np.float32)
    com["conv_w_in"] = np.ascontiguousarray(inp["conv_w_in"], np.float32)
    com["conv_w_out"] = np.ascontiguousarray(inp["conv_w_out"], np.float32)
    cw = np.asarray(inp["conv_w"], np.float32)
    cbias = np.asarray(inp["conv_b"], np.float32)
    cp = np.concatenate([cw, cbias[:, None, :]], axis=1)
    com["convp"] = np.ascontiguousarray(cp.reshape(2, 4, NC_, 128).transpose(3, 0, 1, 2))
    com["ident_f"] = np.eye(128, dtype=np.float32)
    com["ident_b"] = np.eye(128, dtype=np.float32).astype(ml_dtypes.bfloat16)
    com["onesD"] = np.full((128, 128), 1.0 / D, np.float32)
    com["ones_f"] = np.ones((128, 128), np.float32)
    com["attn_w_qkv"] = np.ascontiguousarray(inp["attn_w_qkv"], np.float32)
    com["attn_w_o"] = np.ascontiguousarray(inp["attn_w_o"], np.float32)
    qn = np.asarray(inp["attn_q_norm"], np.float32)[0]
    kn = np.asarray(inp["attn_k_norm"], np.float32)[0]
    com["qkn"] = np.ascontiguousarray(np.stack([np.tile(qn, 2), np.tile(kn, 2)], axis=1))
    com["na_w_qkv"] = np.ascontiguousarray(inp["na_w_qkv"], np.float32)
    com["na_w_o"] = np.ascontiguousarray(inp["na_w_o"], np.float32)
    P = np.zeros((128, 128), np.float32)
    for hh in range(2):
        for half in range(2):
            b = hh * 64 + half * 32
            for i in range(16):
                P[b + 16 + i, b + i] = -1.0
                P[b + i, b + 16 + i] = 1.0
    com["perm"] = P
    blk = np.zeros((128, 128), np.float32)
    blk[:64, :64] = 1.0 / 64
    blk[64:, 64:] = 1.0 / 64
    com["blk"] = blk
    sw = np.zeros((128, 128), np.float32)
    for m in range(128):
        sw[(m + 64) % 128, m] = 1.0
    com["swp"] = sw
    return com


def _prep_core(inp, ci):
    d = {}
    if ci < 2:
        tok = np.asarray(inp["x_sample"], np.float32)[ci]
        cond = np.asarray(inp["c"], np.float32)[ci]
        seqlen = 1024
    else:
        s0 = 4 * (ci - 2)
        tok = np.asarray(inp["x_prompt"], np.float32)[s0:s0 + 4].reshape(T, D)
        cond = np.asarray(inp["c_ctx"], np.float32)
        seqlen = 256
    d["xT_in"] = np.ascontiguousarray(tok.T)
    d["cond"] = np.ascontiguousarray(cond.reshape(NC_, 128).T)
    t = np.arange(T)
    ml = (t % seqlen != 0).astype(np.float32)
    mr = (t % seqlen != seqlen - 1).astype(np.float32)
    d["cmask"] = np.ascontiguousarray(np.broadcast_to(np.stack([ml, mr])[None], (128, 2, T)))
    bf = ml_dtypes.bfloat16
    k = np.arange(1280)
    q = np.arange(T)
    tk = k - 256
    if ci < 2:
        inv = (np.float32(10000.0) ** (-np.arange(16, dtype=np.float32) / np.float32(16))).astype(np.float32)
        row = (t // 64).astype(np.float32)
        col = (t % 64).astype(np.float32)
        cos = np.zeros((128, T), np.float32)
        sin = np.zeros((128, T), np.float32)
        for p in range(128):
            dd = p % 64
            pos = row if dd < 32 else col
            ang = (pos * inv[(dd % 32) % 16]).astype(np.float32)
            cos[p] = np.cos(ang)
            sin[p] = np.sin(ang)
        d["rope"] = np.ascontiguousarray(np.stack([cos, sin], axis=1))
        ck = np.asarray(inp["cache_k_attn"], np.float32)[ci, 0]
        d["ckT_attn"] = np.ascontiguousarray(np.tile(ck.transpose(2, 1, 0), (2, 1, 1)))
        d["cv_attn"] = np.ascontiguousarray(np.asarray(inp["cache_v_attn"], np.float32)[ci, 0].reshape(256, 256))
        d["ckT_na"] = np.ascontiguousarray(np.asarray(inp["cache_k_na"], np.float32)[ci, 0].reshape(256, D).T)
        d["cv_na"] = np.ascontiguousarray(np.asarray(inp["cache_v_na"], np.float32)[ci, 0].reshape(256, D))
        am = np.zeros((1280, T), np.float32)
        kr, kc = tk // 64, tk % 64
        qr, qc = q // 64, q % 64
        rs = np.clip(qr - 4, 0, 8)
        cs = np.clip(qc - 8, 0, 48)
        ok = ((kr[:, None] >= rs[None]) & (kr[:, None] < rs[None] + 8) &
              (kc[:, None] >= cs[None]) & (kc[:, None] < cs[None] + 16))
        nm = np.where(ok, 0.0, NEG).astype(np.float32)
        nm[:256] = 0.0
        rpb = np.asarray(inp["na_rpb"], np.float32)[0]
        cb2 = np.zeros((16, 2, 64, 16, 64), np.float32)
        kcs = np.arange(64)
        co = kcs[:, None] - kcs[None, :] + 15
        cok = (co >= 0) & (co <= 30)
        coc = np.clip(co, 0, 30)
        for dr in range(2):
            for jj in range(16):
                ro = 14 - jj + dr
                if 0 <= ro <= 14:
                    cb2[:, dr, :, jj, :] = np.where(cok[None], rpb[:, ro][:, coc], 0.0)
        d["cb2"] = np.ascontiguousarray(cb2.reshape(16, 128, 1024)).astype(bf)
    else:
        d["rope"] = np.ascontiguousarray(np.stack([np.ones((128, T), np.float32), np.zeros((128, T), np.float32)], axis=1))
        d["ckT_attn"] = np.zeros((128, 4, 256), np.float32)
        d["cv_attn"] = np.zeros((256, 256), np.float32)
        d["ckT_na"] = np.zeros((D, 256), np.float32)
        d["cv_na"] = np.zeros((256, D), np.float32)
        ok = (tk[:, None] // 256) == (q[None] // 256)
        am = np.where(ok, 0.0, NEG).astype(np.float32)
        am[:256] = NEG
        nm = am
        d["cb2"] = np.zeros((16, 128, 1024), bf)
    d["amask"] = np.ascontiguousarray(am.reshape(10, 128, T).transpose(1, 0, 2)).astype(bf)
    d["nmask"] = np.ascontiguousarray(nm.reshape(10, 128, T).transpose(1, 0, 2)).astype(bf)
    return d


_PROG_CACHE = {}


def _get_prog(layers):
    key = tuple(layers)
    if key not in _PROG_CACHE:
        _PROG_CACHE[key] = build_program(list(layers))
    return _PROG_CACHE[key]


def kernel(**inputs):
    layers = [(l, l % 3, l // 3) for l in range(DEPTH)]
    nc = _get_prog(layers)
    com = _prep_common(inputs)
    in_maps = []
    for ci in range(8):
        d = dict(com)
        d.update(_prep_core(inputs, min(ci, NCORES - 1)))
        in_maps.append(d)
    res = run_bass_kernel_spmd(nc, in_maps, core_ids=list(range(8)))
    R_ = res.results
    y_s = np.stack([np.asarray(R_[ci]["yT_out"]).T for ci in range(2)]).astype(np.float32)
    y_p = np.concatenate([np.asarray(R_[ci]["yT_out"]).T.reshape(4, 256, D) for ci in range(2, 6)]).astype(np.float32)
    nk_a = np.concatenate([np.asarray(R_[ci]["kT_attn_out"]).T.reshape(4, 256, 4, 64) for ci in range(2, 6)])
    nv_a = np.concatenate([np.asarray(R_[ci]["v_attn_out"]).reshape(4, 256, 4, 64) for ci in range(2, 6)])
    nk_n = np.concatenate([np.asarray(R_[ci]["kT_na_out"]).T.reshape(4, 256, 16, 64) for ci in range(2, 6)])
    nv_n = np.concatenate([np.asarray(R_[ci]["v_na_out"]).reshape(4, 256, 16, 64) for ci in range(2, 6)])
    f = lambda a: np.ascontiguousarray(a[:, None], dtype=np.float32)
    return (np.ascontiguousarray(y_p), np.ascontiguousarray(y_s), f(nk_a), f(nv_a), f(nk_n), f(nv_n))
```

```python
import numpy as np
import ml_dtypes
from contextlib import ExitStack
import concourse.bass as bass
import concourse.mybir as mybir
from concourse.bass_utils import run_bass_kernel_spmd

F32 = mybir.dt.float32
BF16 = mybir.dt.bfloat16
ALU = mybir.AluOpType
AF = mybir.ActivationFunctionType

D = 1024
T = 1024
NC_ = 8
NCORES = 6
DEPTH = 4
NE = 32
ALPHA = (2 * DEPTH) ** 0.25
BETA = (8 * DEPTH) ** -0.25
LN_EPS = 1e-5
RMS_EPS = 1e-6
NEG = -30000.0
S7 = float(1.0 / (1.0 + np.exp(-1.702 * 7.0)))
import os
CAP = int(os.environ.get('KCAP', '4000'))
STAGE = int(os.environ.get('KSTAGE', '9'))
LD = int(os.environ.get('KLD', '4'))
NED = int(os.environ.get('KNED', '32'))
SUB = int(os.environ.get('KSUB', '9'))
KDV = int(os.environ.get('KDV', '9'))


class Sched:
    def __init__(self):
        self.ops = []
        self.lastw = {}
        self.rd_eng = {}
        self.rd_dma = {}
        self.seen = set()
        self.seen_order = []
        self.scopes = []
        self.fence = set()
        self.last_eng = {}
        self.last_dma = {}

    def mark(self):
        self.scopes.append(len(self.seen_order))

    def release(self):
        n = self.scopes.pop()
        for x in self.seen_order[n:]:
            self.seen.discard(x)
        del self.seen_order[n:]
        self.fence = set(self.last_eng.values()) | set(self.last_dma.values())

    def add(self, eng, fn, r=(), w=(), dma=None):
        deps = set()
        for x in list(r) + list(w):
            if x not in self.seen:
                self.seen.add(x)
                self.seen_order.append(x)
                deps |= self.fence
        for x in r:
            if x in self.lastw:
                deps.add(self.lastw[x])
        for x in w:
            if x in self.lastw:
                deps.add(self.lastw[x])
            deps.update(self.rd_eng.get(x, {}).values())
            deps.update(self.rd_dma.get(x, ()))
        i = len(self.ops)
        self.ops.append(dict(eng=eng, fn=fn, deps=deps, dma=dma))
        if dma is None:
            self.last_eng[eng] = i
        else:
            self.last_dma[dma] = i
        for x in r:
            if dma is None:
                self.rd_eng.setdefault(x, {})[eng] = i
            else:
                self.rd_dma.setdefault(x, []).append(i)
        for x in w:
            self.lastw[x] = i
            self.rd_eng[x] = {}
            self.rd_dma[x] = []
        return i

    def emit(self, nc, final_waits):
        ops = self.ops
        needed = set()
        for op in ops:
            for d_ in op['deps']:
                if op['eng'] == 'pe' and op['dma'] is None and ops[d_]['eng'] == 'pe' and ops[d_]['dma'] is None:
                    continue
                needed.add(d_)
        for i in final_waits:
            needed.add(i)
        engs = ['pe', 'act', 'dve', 'pool', 'sp']
        seq = {e: 0 for e in engs}
        dcount = {}
        ev = {}
        for i, op in enumerate(ops):
            if op['dma'] is not None:
                n = dcount.get(op['dma'], 0)
                dcount[op['dma']] = n + 1
                ev[i] = (('d', op['dma']), 16 * (n + 1))
            elif i in needed:
                e = op['eng']
                s = seq[e]
                seq[e] = s + 1
                ev[i] = (('e', e, s // CAP), s % CAP + 1)
        semkeys = []
        for i in sorted(ev):
            if ev[i][0] not in semkeys:
                semkeys.append(ev[i][0])
        with ExitStack() as es:
            sems = {}
            for k in semkeys:
                sems[k] = es.enter_context(nc.semaphore("s_" + "_".join(str(x) for x in k[1:]).replace(" ", "")
                                                        .replace("(", "").replace(")", "").replace(",", "_").replace("'", "")))
            block = es.enter_context(nc.Block())
            per = {e: [i for i, op in enumerate(ops) if op['eng'] == e] for e in engs}

            def run(e, eng):
                waited = {}
                for i in per[e]:
                    op = ops[i]
                    for d in sorted(op['deps']):
                        if d not in ev:
                            continue
                        if ops[d]['eng'] == 'pe' and e == 'pe' and ops[d]['dma'] is None:
                            continue
                        k, v = ev[d]
                        if waited.get(k, 0) < v:
                            eng.wait_ge(sems[k], v)
                            waited[k] = v
                    ins = op['fn'](eng)
                    if i in ev:
                        k, v = ev[i]
                        ins.then_inc(sems[k], 16 if k[0] == 'd' else 1)
                if e == 'sp':
                    for i in final_waits:
                        k, v = ev[i]
                        eng.wait_ge(sems[k], v)

            block.tensor(lambda eng: run('pe', eng))
            block.scalar(lambda eng: run('act', eng))
            block.vector(lambda eng: run('dve', eng))
            block.gpsimd(lambda eng: run('pool', eng))
            block.sync(lambda eng: run('sp', eng))


class Arena:
    def __init__(self, ap, words):
        self.ap = ap
        self.words = words
        self.top = 0
        self.marks = []
        self.sched = None

    def alloc(self, words):
        words = (words + 7) // 8 * 8
        o = self.top
        self.top += words
        assert self.top <= self.words, (self.top, self.words)
        return o

    def f32(self, shape):
        n = int(np.prod(shape[1:]))
        o = self.alloc(n)
        v = self.ap[0:shape[0], o:o + n]
        if len(shape) == 3:
            v = v.rearrange("p (a b) -> p a b", a=shape[1])
        elif len(shape) == 4:
            v = v.rearrange("p (a b c) -> p a b c", a=shape[1], b=shape[2])
        return v

    def bf16(self, shape):
        n = int(np.prod(shape[1:]))
        o = self.alloc((n + 1) // 2)
        v = self.ap[0:shape[0], o:o + (n + 1) // 2].bitcast(BF16)[:, 0:n]
        if len(shape) == 3:
            v = v.rearrange("p (a b) -> p a b", a=shape[1])
        elif len(shape) == 4:
            v = v.rearrange("p (a b c) -> p a b c", a=shape[1], b=shape[2])
        return v

    def _shape(self, v, shape):
        if len(shape) == 3:
            v = v.rearrange("p (a b) -> p a b", a=shape[1])
        elif len(shape) == 4:
            v = v.rearrange("p (a b c) -> p a b c", a=shape[1], b=shape[2])
        return v

    def f32_at(self, o, shape):
        n = int(np.prod(shape[1:]))
        return self._shape(self.ap[0:shape[0], o:o + n], shape)

    def bf16_at(self, o, shape):
        n = int(np.prod(shape[1:]))
        return self._shape(self.ap[0:shape[0], o:o + (n + 1) // 2].bitcast(BF16)[:, 0:n], shape)

    def mark(self):
        self.marks.append(self.top)
        if self.sched is not None:
            self.sched.mark()

    def release(self):
        self.top = self.marks.pop()
        if self.sched is not None:
            self.sched.release()


def build_program(layers, debug_out=False):
    nc = bass.Bass("TRN2", target_bir_lowering=False)
    S = Sched()
    L = DEPTH

    def din(name, shape, dt=F32):
        return nc.dram_tensor(name, list(shape), dt, kind="ExternalInput").ap()

    def dout(name, shape, dt=F32):
        return nc.dram_tensor(name, list(shape), dt, kind="ExternalOutput").ap()

    xT_in = din("xT_in", [D, T])
    cond_in = din("cond", [128, NC_])
    w_mod = din("w_mod", [LD, D, 6 * D])
    bmod_in = din("bmod", [128, L, 48])
    lnp_in = din("lnp", [128, L, 4, NC_])
    router_w = din("router_w", [L, D, NE])
    router_b = din("router_b", [L, NE])
    moe_w1 = din("moe_w1", [LD, NED, D, 2 * D])
    moe_w2 = din("moe_w2", [LD, NED, D, D])
    b1_in = din("b1", [128, L, NE, 16])
    moe_b2 = din("moe_b2", [L, NE, D])
    conv_w_in = din("conv_w_in", [2, D, 3 * D])
    conv_w_out = din("conv_w_out", [2, D, D])
    convp_in = din("convp", [128, 2, 4, NC_])
    cmask_in = din("cmask", [128, 2, T])
    ident_f_in = din("ident_f", [128, 128])
    ident_b_in = din("ident_b", [128, 128], BF16)
    onesD_in = din("onesD", [128, 128])
    ones_in = din("ones_f", [128, 128])
    attn_w_qkv = din("attn_w_qkv", [1, D, 1536])
    attn_w_o = din("attn_w_o", [1, D, D])
    qkn_in = din("qkn", [128, 2])
    rope_in = din("rope", [128, 2, T])
    perm_in = din("perm", [128, 128])
    blk_in = din("blk", [128, 128])
    ckT_attn = din("ckT_attn", [128, 4, 256])
    swp_in = din("swp", [128, 128])
    cv_attn = din("cv_attn", [256, 256])
    amask_in = din("amask", [128, 10, T], BF16)
    na_w_qkv = din("na_w_qkv", [1, D, 3 * D])
    na_w_o = din("na_w_o", [1, D, D])
    ckT_na = din("ckT_na", [D, 256])
    cv_na = din("cv_na", [256, D])
    nmask_in = din("nmask", [128, 10, T], BF16)
    cb2_in = din("cb2", [16, 128, 16 * 64], BF16)

    yT_out = dout("yT_out", [D, T])
    kT_attn_out = dout("kT_attn_out", [256, T])
    v_attn_out = dout("v_attn_out", [T, 256])
    kT_na_out = dout("kT_na_out", [D, T])
    v_na_out = dout("v_na_out", [T, D])

    AW = 53100
    es = ExitStack()
    arena_t = es.enter_context(nc.sbuf_tensor("arena", [128, AW], F32))
    A = Arena(arena_t, AW)
    A.sched = S
    PS = [es.enter_context(nc.psum_tensor("ps%d" % i, [128, 512], F32)) for i in range(8)]

    def ps(i):
        return PS[i][:, :]

    def psr(i):
        return ("ps", i)

    final_waits = []

    xT = A.f32([128, NC_, T])
    hb = A.bf16([128, NC_, T])
    R = A.f32([128, NC_, T])
    ident_f = A.f32([128, 128])
    onesD = A.f32([128, 128])
    ones_f = A.f32([128, 128])
    ident_b = A.bf16([128, 128])
    condT = A.f32([128, NC_])
    scT = A.bf16([128, NC_])
    bmod = A.f32([128, L, 48])
    lnp = A.f32([128, L, 4, NC_])
    convp = A.f32([128, 2, 4, NC_])
    modT = A.f32([128, 48])
    SQ = [A.f32([128, 512]) for _ in range(2)]
    RS = A.f32([128, 512])

    def sp_load(dst, src, name, eng='sp', res=None):
        S.add(eng, lambda e, d=dst, s=src: e.dma_start(out=d, in_=s), w=(res or [name]), dma=name)

    sp_load(xT, xT_in.rearrange("(c p) t -> p c t", p=128), "xT", res=[("xT", c) for c in range(NC_)])
    sp_load(condT, cond_in, "condT")
    sp_load(ident_f, ident_f_in, "ident_f")
    sp_load(onesD, onesD_in, "onesD")
    sp_load(ones_f, ones_in, "ones_f")
    sp_load(ident_b, ident_b_in, "ident_b")
    sp_load(bmod, bmod_in, "bmod")
    sp_load(lnp, lnp_in, "lnp")
    sp_load(convp, convp_in, "convp")
    S.add('act', lambda e: e.activation(out=scT, in_=condT, func=AF.Silu), r=["condT"], w=["scT"])

    def adaln(l):
        A.mark()
        Wm = [A.bf16([128, NC_, 1024]) for _ in range(2)]
        for g in range(6):
            wb = Wm[g % 2]
            nm = ("Wm", g % 2)
            S.add('pool', lambda e, wb=wb, g=g: e.dma_start(
                out=wb, in_=w_mod[l, :, g * 1024:(g + 1) * 1024].rearrange("(c p) f -> p c f", p=128)),
                w=[nm], dma=nm)
            for kk in range(8):
                k = g * 8 + kk
                for c in range(NC_):
                    S.add('pe', lambda e, wb=wb, kk=kk, c=c, k=k: e.matmul(
                        ps(7)[:, k:k + 1], lhsT=wb[:, c, kk * 128:(kk + 1) * 128], rhs=scT[:, c:c + 1],
                        start=(c == 0), stop=(c == NC_ - 1)),
                        r=[nm, "scT"], w=[psr(7)])
        S.add('dve', lambda e: e.tensor_tensor(out=modT, in0=ps(7)[:, 0:48], in1=bmod[:, l, :], op=ALU.add),
              r=[psr(7), "bmod"], w=["modT"])
        for lo in (8, 32):
            S.add('dve', lambda e, lo=lo: e.tensor_scalar_add(out=modT[:, lo:lo + 8], in0=modT[:, lo:lo + 8], scalar1=1.0),
                  r=["modT"], w=["modT"])
        for lo in (16, 40):
            S.add('dve', lambda e, lo=lo: e.tensor_scalar_mul(out=modT[:, lo:lo + 8], in0=modT[:, lo:lo + 8],
                                                                scalar1=1.0 / ALPHA), r=["modT"], w=["modT"])
        A.release()

    def modulate(src, off):
        for c in range(NC_):
            S.add('dve', lambda e, c=c: e.tensor_scalar(
                out=hb[:, c, :], in0=src[:, c, :], scalar1=modT[:, off + 8 + c:off + 9 + c],
                scalar2=modT[:, off + c:off + c + 1], op0=ALU.mult, op1=ALU.add),
                r=["modT", ("xT", c)], w=[("hb", c)])

    def layernorm(l, which, mod_off, h32=None):
        gi, bi = (0, 1) if which == 1 else (2, 3)
        eps = LN_EPS / (ALPHA * ALPHA)
        for th in range(2):
            cs = slice(th * 512, (th + 1) * 512)
            pm, pv = 5, 6
            for c in range(NC_):
                S.add('pe', lambda e, c=c, cs=cs: e.matmul(ps(pm), lhsT=onesD, rhs=R[:, c, cs],
                                                           start=(c == 0), stop=(c == NC_ - 1)),
                      r=["onesD", ("R", c, th)], w=[psr(pm)])
            for c in range(NC_):
                S.add('dve', lambda e, c=c, cs=cs: e.tensor_tensor(out=R[:, c, cs], in0=R[:, c, cs], in1=ps(pm),
                                                                   op=ALU.subtract),
                      r=[psr(pm), ("R", c, th)], w=[("R", c, th)])
            for c in range(NC_):
                sq = SQ[c % 2]
                S.add('act', lambda e, c=c, cs=cs, sq=sq: e.activation(out=sq, in_=R[:, c, cs], func=AF.Square),
                      r=[("R", c, th)], w=[("SQ", c % 2)])
                S.add('pe', lambda e, c=c, sq=sq: e.matmul(ps(pv), lhsT=onesD, rhs=sq,
                                                           start=(c == 0), stop=(c == NC_ - 1)),
                      r=["onesD", ("SQ", c % 2)], w=[psr(pv)])
            S.add('dve', lambda e: e.tensor_scalar_add(out=RS, in0=ps(pv), scalar1=eps), r=[psr(pv)], w=["RS"])
            S.add('act', lambda e: e.activation(out=RS, in_=RS, func=AF.Sqrt), r=["RS"], w=["RS"])
            S.add('dve', lambda e: e.reciprocal(out=RS, in_=RS), r=["RS"], w=["RS"])
            for c in range(NC_):
                S.add('dve', lambda e, c=c, cs=cs: e.tensor_tensor(out=R[:, c, cs], in0=R[:, c, cs], in1=RS, op=ALU.mult),
                      r=["RS", ("R", c, th)], w=[("R", c, th)])
                S.add('dve', lambda e, c=c, cs=cs: e.tensor_scalar(
                    out=xT[:, c, cs], in0=R[:, c, cs], scalar1=lnp[:, l, gi, c:c + 1], scalar2=lnp[:, l, bi, c:c + 1],
                    op0=ALU.mult, op1=ALU.add), r=["lnp", ("R", c, th)], w=[("xT", c), ("xTh", c, th)])
                if mod_off is not None:
                    S.add('dve', lambda e, c=c, cs=cs: e.tensor_scalar(
                        out=hb[:, c, cs], in0=xT[:, c, cs], scalar1=modT[:, mod_off + 8 + c:mod_off + 9 + c],
                        scalar2=modT[:, mod_off + c:mod_off + c + 1], op0=ALU.mult, op1=ALU.add),
                        r=["modT", ("xTh", c, th)], w=[("hb", c)])
                    if h32 is not None:
                        S.add('pool', lambda e, c=c, cs=cs: e.tensor_scalar(
                            out=h32[:, c, cs], in0=xT[:, c, cs], scalar1=modT[:, mod_off + 8 + c:mod_off + 9 + c],
                            scalar2=modT[:, mod_off + c:mod_off + c + 1], op0=ALU.mult, op1=ALU.add),
                            r=["modT", ("xTh", c, th)], w=[("h32", c), ("W1", 1)])

    def residual_from_psum(c, th, pbank, gate_off):
        cs = slice(th * 512, (th + 1) * 512)
        S.add('dve', lambda e: e.scalar_tensor_tensor(
            out=R[:, c, cs], in0=ps(pbank), scalar=modT[:, gate_off + c:gate_off + c + 1], in1=xT[:, c, cs],
            op0=ALU.mult, op1=ALU.add), r=[psr(pbank), "modT", ("xT", c)], w=[("R", c, th)])

    def conv_mixer(l, j):
        A.mark()
        Wi = [A.bf16([128, NC_, 1024]) for _ in range(2)]
        Wo = A.bf16([128, NC_, 1024])
        U = A.f32([128, NC_, T])
        cmask = A.f32([128, 2, T])
        V = A.bf16([128, NC_, T])
        TMP = [A.f32([128, 512]) for _ in range(2)]
        sp_load(cmask, cmask_in, "cmask")
        for pi, part in enumerate((1, 2, 0)):
            wb = Wi[pi % 2]
            nm = ("Wi", pi % 2)
            S.add('pool', lambda e, wb=wb, part=part: e.dma_start(
                out=wb, in_=conv_w_in[j, :, part * 1024:(part + 1) * 1024].rearrange("(c p) f -> p c f", p=128)),
                w=[nm], dma=nm)
            if pi == 0:
                S.add('pool', lambda e: e.dma_start(out=Wo, in_=conv_w_out[j].rearrange("(c p) f -> p c f", p=128)),
                      w=["Wo"], dma="Wo")
            for fc in range(NC_):
                for th in range(2):
                    cs = slice(th * 512, (th + 1) * 512)
                    pb = (fc * 2 + th) % 4
                    for c in range(NC_):
                        S.add('pe', lambda e, wb=wb, fc=fc, c=c, cs=cs, pb=pb: e.matmul(
                            ps(pb), lhsT=wb[:, c, fc * 128:(fc + 1) * 128], rhs=hb[:, c, cs],
                            start=(c == 0), stop=(c == NC_ - 1)), r=[nm, ("hb", c)], w=[psr(pb)])
                    if part == 1:
                        S.add('act', lambda e, fc=fc, cs=cs, pb=pb: e.copy(out=U[:, fc, cs], in_=ps(pb)),
                              r=[psr(pb)], w=[("U", fc, th)])
                    elif part == 2:
                        S.add('dve', lambda e, fc=fc, cs=cs, pb=pb: e.tensor_tensor(
                            out=U[:, fc, cs], in0=U[:, fc, cs], in1=ps(pb), op=ALU.mult),
                            r=[psr(pb), ("U", fc, th)], w=[("U", fc, th)])
                    else:
                        tmp = TMP[th]
                        tn = ("TMP", th)
                        lo = th * 512
                        S.add('dve', lambda e, fc=fc, cs=cs, tmp=tmp: e.tensor_scalar(
                            out=tmp, in0=U[:, fc, cs], scalar1=convp[:, j, 1, fc:fc + 1], scalar2=convp[:, j, 3, fc:fc + 1],
                            op0=ALU.mult, op1=ALU.add), r=[("U", fc, 0), ("U", fc, 1), "convp"], w=[tn])
                        a0 = max(lo - 1, 0)
                        n0 = 512 - (1 if th == 0 else 0)
                        d0 = 1 if th == 0 else 0
                        A.mark()
                        S.add('pool', lambda e, fc=fc, a0=a0, n0=n0, lo=lo, d0=d0, th=th: e.tensor_tensor(
                            out=SQ[th][:, d0:d0 + n0], in0=U[:, fc, a0:a0 + n0], in1=cmask[:, 0, lo + d0:lo + d0 + n0],
                            op=ALU.mult), r=[("U", fc, 0), ("U", fc, 1), "cmask"], w=[("SQ", th)])
                        S.add('dve', lambda e, fc=fc, d0=d0, n0=n0, tmp=tmp, th=th: e.scalar_tensor_tensor(
                            out=tmp[:, d0:d0 + n0], in0=SQ[th][:, d0:d0 + n0], scalar=convp[:, j, 0, fc:fc + 1],
                            in1=tmp[:, d0:d0 + n0], op0=ALU.mult, op1=ALU.add), r=[("SQ", th), tn, "convp"], w=[tn])
                        n1 = 512 - (1 if th == 1 else 0)
                        S.add('pool', lambda e, fc=fc, lo=lo, n1=n1, th=th: e.tensor_tensor(
                            out=SQ[th][:, 0:n1], in0=U[:, fc, lo + 1:lo + 1 + n1], in1=cmask[:, 1, lo:lo + n1],
                            op=ALU.mult), r=[("U", fc, 0), ("U", fc, 1), "cmask", tn], w=[("SQ", th)])
                        S.add('dve', lambda e, fc=fc, n1=n1, tmp=tmp, th=th: e.scalar_tensor_tensor(
                            out=tmp[:, 0:n1], in0=SQ[th][:, 0:n1], scalar=convp[:, j, 2, fc:fc + 1],
                            in1=tmp[:, 0:n1], op0=ALU.mult, op1=ALU.add), r=[("SQ", th), tn, "convp"], w=[tn])
                        A.release()
                        S.add('dve', lambda e, fc=fc, cs=cs, tmp=tmp, pb=pb: e.tensor_tensor(
                            out=V[:, fc, cs], in0=tmp, in1=ps(pb), op=ALU.mult), r=[tn, psr(pb)], w=[("V", fc)])
        for fc in range(NC_):
            for th in range(2):
                cs = slice(th * 512, (th + 1) * 512)
                pb = (fc * 2 + th) % 4
                for c in range(NC_):
                    S.add('pe', lambda e, fc=fc, c=c, cs=cs, pb=pb: e.matmul(
                        ps(pb), lhsT=Wo[:, c, fc * 128:(fc + 1) * 128], rhs=V[:, c, cs],
                        start=(c == 0), stop=(c == NC_ - 1)), r=["Wo", ("V", c)], w=[psr(pb)])
                residual_from_psum(fc, th, pb, 16)
        A.release()


    def attention(l, j, is_na):
        nvh = 16 if is_na else 4
        NV = nvh * 64
        ng = 8 if is_na else 4
        wqkv = na_w_qkv if is_na else attn_w_qkv
        wo_d = na_w_o if is_na else attn_w_o
        mask_in = nmask_in if is_na else amask_in
        kT_out = kT_na_out if is_na else kT_attn_out
        v_out = v_na_out if is_na else v_attn_out
        A.mark()
        QT = A.bf16([128, NC_, T])
        KT = A.bf16([128, ng, 1280])
        VA = A.bf16([128, 10, nvh, 65])
        OTK = A.bf16([128, 8, D])
        REC = A.f32([128, 8])
        qk2 = A.f32([128, 2])
        A.mark()
        WP = [A.bf16([128, NC_, 512]) for _ in range(2)]
        KST = [A.f32([128, 512]) for _ in range(2)]
        VST = [A.f32([128, NV]) for _ in range(2)]
        if not is_na:
            ROPE = A.f32([128, 2, T])
            PERM = A.f32([128, 128])
            BLK = A.f32([128, 128])
            SWP = A.f32([128, 128])
            QN = A.f32([128, 512])
            T1 = A.f32([128, 512])
            T2 = A.f32([128, 512])
            sp_load(ROPE, rope_in, "ROPE")
            sp_load(PERM, perm_in, "PERM")
            sp_load(BLK, blk_in, "BLK")
            sp_load(SWP, swp_in, "SWP")
            sp_load(qk2, qkn_in, "qk2")
            S.add('dve', lambda e: e.tensor_scalar_mul(out=qk2[:, 0:1], in0=qk2[:, 0:1], scalar1=0.125),
                  r=["qk2"], w=["qk2"])
        S.add('dve', lambda e: e.memset(VA[:, :, :, 64:65], 1.0), w=[("VA1",)])
        if is_na:
            S.add('pool', lambda e: e.dma_start(out=KT[:, :, 0:256], in_=ckT_na.rearrange("(c p) t -> p c t", p=128)),
                  w=[("KTc",)], dma="KTc")
            for blk_ in range(2):
                S.add('pool', lambda e, blk_=blk_: e.dma_start(
                    out=VA[:, blk_, :, 0:64],
                    in_=cv_na[blk_ * 128:(blk_ + 1) * 128, :].rearrange("p (h d) -> p h d", d=64)),
                    w=[("VAc", blk_)], dma=("VAc", blk_))
        else:
            S.add('pool', lambda e: e.dma_start(out=KT[:, :, 0:256], in_=ckT_attn), w=[("KTc",)], dma="KTc")
            for blk_ in range(2):
                S.add('pool', lambda e, blk_=blk_: e.dma_start(
                    out=VA[:, blk_, :, 0:64],
                    in_=cv_attn[blk_ * 128:(blk_ + 1) * 128, :].rearrange("p (h d) -> p h d", d=64)),
                    w=[("VAc", blk_)], dma=("VAc", blk_))
        npieces = 6 if is_na else 3
        kst_i = 0
        for pi in range(npieces):
            wb = WP[pi % 2]
            nm = ("WP", pi % 2)
            S.add('pool', lambda e, wb=wb, pi=pi: e.dma_start(
                out=wb, in_=wqkv[j, :, pi * 512:(pi + 1) * 512].rearrange("(c p) f -> p c f", p=128)),
                w=[nm], dma=nm)
            if pi < 2:
                fm = [("q", pi * 4 + f, f) for f in range(4)]
                vcols = None
            elif is_na and pi < 4:
                fm = [("k", (pi - 2) * 4 + f, f) for f in range(4)]
                vcols = None
            elif is_na:
                fm = []
                vcols = (0, 512, (pi - 4) * 512)
            else:
                fm = [("k", f, f) for f in range(2)]
                vcols = (256, 256, 0)
            for (kind_, gc, fl) in fm:
                for th in range(2):
                    cs = slice(th * 512, (th + 1) * 512)
                    pb = (fl * 2 + th) % 4
                    for c in range(NC_):
                        S.add('pe', lambda e, wb=wb, fl=fl, c=c, cs=cs, pb=pb: e.matmul(
                            ps(pb), lhsT=wb[:, c, fl * 128:(fl + 1) * 128], rhs=hb[:, c, cs],
                            start=(c == 0), stop=(c == NC_ - 1)), r=[nm, ("hb", c)], w=[psr(pb)])
                    if is_na:
                        if kind_ == "q":
                            S.add('act', lambda e, gc=gc, cs=cs, pb=pb: e.mul(out=QT[:, gc, cs], in_=ps(pb), mul=0.125),
                                  r=[psr(pb)], w=[("QT", gc)])
                        else:
                            kst = KST[kst_i % 2]
                            kn = ("KST", kst_i % 2)
                            kst_i += 1
                            S.add('act', lambda e, kst=kst, pb=pb: e.copy(out=kst, in_=ps(pb)), r=[psr(pb)], w=[kn])
                            S.add('dve', lambda e, kst=kst, gc=gc, th=th: e.tensor_copy(
                                out=KT[:, gc, 256 + th * 512:256 + (th + 1) * 512], in_=kst), r=[kn], w=[("KT", gc)])
                            S.add('sp', lambda e, kst=kst, gc=gc, cs=cs: e.dma_start(
                                out=kT_out[gc * 128:(gc + 1) * 128, cs], in_=kst), r=[kn], w=[("kout", gc, th)], dma=kn)
                    else:
                        x = th
                        sq = SQ[x]
                        S.add('act', lambda e, sq=sq, pb=pb: e.activation(out=sq, in_=ps(pb), func=AF.Square),
                              r=[psr(pb)], w=[("SQ", x)])
                        S.add('pe', lambda e, sq=sq: e.matmul(ps(4), lhsT=BLK, rhs=sq, start=True, stop=True),
                              r=["BLK", ("SQ", x)], w=[psr(4)])
                        S.add('dve', lambda e: e.tensor_scalar_add(out=RS, in0=ps(4), scalar1=RMS_EPS), r=[psr(4)], w=["RS"])
                        S.add('act', lambda e: e.activation(out=RS, in_=RS, func=AF.Sqrt), r=["RS"], w=["RS"])
                        S.add('dve', lambda e: e.reciprocal(out=RS, in_=RS), r=["RS"], w=["RS"])
                        gcol = 0 if kind_ == "q" else 1
                        S.add('dve', lambda e, pb=pb, gcol=gcol: e.scalar_tensor_tensor(
                            out=QN, in0=ps(pb), scalar=qk2[:, gcol:gcol + 1], in1=RS, op0=ALU.mult, op1=ALU.mult),
                            r=[psr(pb), "qk2", "RS"], w=["QN"])
                        S.add('pe', lambda e: e.matmul(ps(5), lhsT=PERM, rhs=QN, start=True, stop=True),
                              r=["PERM", "QN"], w=[psr(5)])
                        S.add('pool', lambda e, cs=cs: e.tensor_tensor(out=T1, in0=QN, in1=ROPE[:, 0, cs], op=ALU.mult),
                              r=["QN", "ROPE"], w=["T1"])
                        S.add('dve', lambda e, cs=cs: e.tensor_tensor(out=T2, in0=ROPE[:, 1, cs], in1=ps(5), op=ALU.mult),
                              r=[psr(5), "ROPE"], w=["T2"])
                        if kind_ == "q":
                            S.add('dve', lambda e, gc=gc, cs=cs: e.tensor_tensor(out=QT[:, gc, cs], in0=T1, in1=T2, op=ALU.add),
                                  r=["T1", "T2"], w=[("QT", gc)])
                        else:
                            kst = KST[kst_i % 2]
                            kn = ("KST", kst_i % 2)
                            kst_i += 1
                            kc0 = 256 + th * 512
                            S.add('dve', lambda e, kst=kst: e.tensor_tensor(out=kst, in0=T1, in1=T2, op=ALU.add),
                                  r=["T1", "T2"], w=[kn])
                            S.add('sp', lambda e, kst=kst, gc=gc, cs=cs: e.dma_start(
                                out=kT_out[gc * 128:(gc + 1) * 128, cs], in_=kst), r=[kn], w=[("kout", gc, th)], dma=kn)
                            S.add('act', lambda e, kst=kst, gc=gc, kc0=kc0: e.copy(
                                out=KT[0:64, 2 * gc, kc0:kc0 + 512], in_=kst[0:64, :]), r=[kn], w=[("KT", 2 * gc, 0, th)])
                            S.add('act', lambda e, kst=kst, gc=gc, kc0=kc0: e.copy(
                                out=KT[64:128, 2 * gc + 1, kc0:kc0 + 512], in_=kst[64:128, :]), r=[kn],
                                w=[("KT", 2 * gc + 1, 1, th)])
                            S.add('pe', lambda e, kst=kst: e.matmul(ps(6), lhsT=SWP, rhs=kst, start=True, stop=True),
                                  r=["SWP", kn], w=[psr(6)])
                            S.add('act', lambda e, gc=gc, kc0=kc0: e.copy(
                                out=KT[64:128, 2 * gc, kc0:kc0 + 512], in_=ps(6)[64:128, :]), r=[psr(6)],
                                w=[("KT", 2 * gc, 1, th)])
                            S.add('act', lambda e, gc=gc, kc0=kc0: e.copy(
                                out=KT[0:64, 2 * gc + 1, kc0:kc0 + 512], in_=ps(6)[0:64, :]), r=[psr(6)],
                                w=[("KT", 2 * gc + 1, 0, th)])
            if vcols is not None:
                c0, ncol, vo = vcols
                for tt in range(8):
                    pb = tt % 2
                    ts_ = slice(tt * 128, (tt + 1) * 128)
                    for c in range(NC_):
                        S.add('pe', lambda e, wb=wb, c=c, ts_=ts_, pb=pb, ncol=ncol, c0=c0: e.matmul(
                            ps(pb)[:, 0:ncol], lhsT=hb[:, c, ts_], rhs=wb[:, c, c0:c0 + ncol],
                            start=(c == 0), stop=(c == NC_ - 1)), r=[nm, ("hb", c)], w=[psr(pb)])
                    vst = VST[tt % 2]
                    vn = ("VST", tt % 2)
                    h0 = vo // 64
                    nh = ncol // 64
                    S.add('act', lambda e, vst=vst, pb=pb, vo=vo, ncol=ncol: e.copy(out=vst[:, vo:vo + ncol], in_=ps(pb)[:, 0:ncol]),
                          r=[psr(pb)], w=[vn])
                    S.add('dve', lambda e, tt=tt, pb=pb, h0=h0, nh=nh, ncol=ncol: e.tensor_copy(
                        out=VA[:, 2 + tt, h0:h0 + nh, 0:64],
                        in_=VST[tt % 2][:, h0 * 64:h0 * 64 + ncol].rearrange("p (h d) -> p h d", d=64)),
                        r=[("VST", tt % 2)], w=[("VA", tt, vo)])
                    S.add('sp', lambda e, vst=vst, ts_=ts_, vo=vo, ncol=ncol: e.dma_start(
                        out=v_out[ts_, vo:vo + ncol], in_=vst[:, vo:vo + ncol]), r=[vn], w=[("vout", tt, vo)], dma=vn)
        A.release()
        A.mark()
        MASK = A.bf16([128, 10, T])
        E = A.bf16([128, 10, T])
        CB2 = [A.bf16([128, 16 * 64]) for _ in range(2)] if is_na else None
        sp_load(MASK, mask_in, "MASK")
        qt_all = [("QT", c) for c in range(NC_)]
        kt_all = ([("KT", c) for c in range(ng)] if is_na else
                  [("KT", g_, hf, th) for g_ in range(4) for hf in range(2) for th in range(2)]) + [("KTc",)]
        va_all = [("VA1",), ("VAc", 0), ("VAc", 1)] + [("VA", tt, vo) for tt in range(8) for vo in (range(0, NV, 512) if is_na else [0])]
        for h in range(16):
            hp = (h % 2) * 64
            qc = h // 2
            if is_na:
                ksl = KT[hp:hp + 64, h // 2, :]
                g = h
                cbn = ("CB2", h % 2)
                S.add('sp', lambda e, h=h: e.dma_start(out=CB2[h % 2], in_=cb2_in[h]), w=[cbn], dma=cbn)
            else:
                ksl = KT[hp:hp + 64, h // 4, :]
                g = h // 4
            for b in range(10):
                banks = (0, 1) if b % 2 == 0 else (2, 3)
                for th in range(2):
                    S.add('pe', lambda e, ksl=ksl, b=b, th=th, hp=hp, qc=qc, banks=banks: e.matmul(
                        ps(banks[th]), lhsT=ksl[:, b * 128:(b + 1) * 128],
                        rhs=QT[hp:hp + 64, qc, th * 512:(th + 1) * 512], start=True, stop=False),
                        r=qt_all + kt_all, w=[psr(banks[th])])
                if is_na and b >= 2:
                    bl = b - 2
                    qlo, qhi = max(0, 2 * bl - 7), min(15, 2 * bl + 8)
                    for th in range(2):
                        qa, qb = max(qlo, 8 * th), min(qhi, 8 * th + 7)
                        if qa > qb:
                            continue
                        ja, jb = qa - 2 * bl + 7, qb - 2 * bl + 7
                        S.add('pe', lambda e, th=th, qa=qa, qb=qb, ja=ja, jb=jb, banks=banks, h=h: e.matmul(
                            ps(banks[th])[:, (qa - 8 * th) * 64:(qb + 1 - 8 * th) * 64], lhsT=ident_b,
                            rhs=CB2[h % 2][:, ja * 64:(jb + 1) * 64], start=False, stop=False),
                            r=[cbn, "ident_b"], w=[psr(banks[th])])
                for th in range(2):
                    S.add('pe', lambda e, b=b, th=th, banks=banks: e.matmul(
                        ps(banks[th]), lhsT=ident_b, rhs=MASK[:, b, th * 512:(th + 1) * 512], start=False, stop=True),
                        r=["MASK", "ident_b"], w=[psr(banks[th])])
                for th in range(2):
                    S.add('act', lambda e, b=b, th=th, banks=banks: e.activation(
                        out=E[:, b, th * 512:(th + 1) * 512], in_=ps(banks[th]), func=AF.Exp),
                        r=[psr(banks[th])], w=[("E", b)])
            ob = 4 + 2 * (h % 2)
            for qt in range(8):
                bank = ob + qt // 4
                off = (qt % 4) * 128
                for b in range(10):
                    S.add('pe', lambda e, qt=qt, b=b, bank=bank, off=off, g=g: e.matmul(
                        ps(bank)[:, off:off + 65], lhsT=E[:, b, qt * 128:(qt + 1) * 128], rhs=VA[:, b, g, :],
                        start=(b == 0), stop=(b == 9)), r=[("E", b)] + va_all, w=[psr(bank)])
            for half in range(2):
                bank = ob + half
                S.add('dve', lambda e, bank=bank, half=half: e.reciprocal(
                    out=REC[:, half * 4:(half + 1) * 4],
                    in_=ps(bank).rearrange("p (q c) -> p q c", c=128)[:, :, 64]), r=[psr(bank)], w=[("REC", half)])
                for q4 in range(4):
                    qt = half * 4 + q4
                    S.add('dve', lambda e, bank=bank, q4=q4, qt=qt, h=h: e.tensor_scalar_mul(
                        out=OTK[:, qt, h * 64:(h + 1) * 64], in0=ps(bank)[:, q4 * 128:q4 * 128 + 64],
                        scalar1=REC[:, qt:qt + 1]), r=[psr(bank), ("REC", half)], w=[("OTK", qt)])
        A.release()
        A.mark()
        Wo = A.bf16([128, NC_, 1024])
        S.add('pool', lambda e: e.dma_start(out=Wo, in_=wo_d[j].rearrange("(c p) f -> p c f", p=128)), w=["Wo"], dma="Wo")
        for fc in range(NC_):
            for qg in range(2):
                pb = (fc * 2 + qg) % 4
                pst = PS[pb][:, 0:256].bitcast(BF16)
                for k in range(4):
                    S.add('pe', lambda e, pst=pst, k=k, qg=qg, fc=fc: e.transpose(
                        out=pst[:, k * 128:(k + 1) * 128], in_=OTK[:, qg * 4 + k, fc * 128:(fc + 1) * 128],
                        identity=ident_b), r=[("OTK", qg * 4 + k), "ident_b"], w=[psr(pb)])
                S.add('act', lambda e, pst=pst, fc=fc, qg=qg: e.copy(out=hb[:, fc, qg * 512:(qg + 1) * 512], in_=pst),
                      r=[psr(pb)], w=[("hb", fc)])
        for fc in range(NC_):
            for th in range(2):
                cs = slice(th * 512, (th + 1) * 512)
                pb = (fc * 2 + th) % 4
                for c in range(NC_):
                    S.add('pe', lambda e, fc=fc, c=c, cs=cs, pb=pb: e.matmul(
                        ps(pb), lhsT=Wo[:, c, fc * 128:(fc + 1) * 128], rhs=hb[:, c, cs],
                        start=(c == 0), stop=(c == NC_ - 1)), r=["Wo", ("hb", c)], w=[psr(pb)])
                residual_from_psum(fc, th, pb, 16)
        A.release()
        A.release()

    def moe(l, last):
        A.mark()
        o_w1 = [A.alloc(NC_ * 2048 // 2) for _ in range(2)]
        W1 = [A.bf16_at(o, [128, NC_, 2048]) for o in o_w1]
        h32 = A.f32_at(o_w1[1], [128, NC_, T])
        W2 = A.bf16([128, NC_, 1024])
        aT = A.bf16([128, NC_, T])
        GS = RS
        US = A.f32([128, 512])
        SIG = SQ
        CB = A.f32([128, T])
        b1 = A.f32([128, NE, 16])
        b1s = A.f32([128, NE, 8])
        rw = A.f32([128, NC_, NE])
        rb = A.f32([1, NE])
        o_cb = A.alloc(T)
        combT = A.f32_at(o_cb, [32, T])
        cm = A.f32([32, T])
        b2 = cm
        lg = A.f32([128, NE])
        mx8 = A.f32([128, 8])
        ex = A.f32([128, NE])
        sm = A.f32([128, 4])
        cst = A.f32([128, 4])
        for ci_, val in enumerate((7.0, 8.0, S7, -6.0)):
            S.add('dve', lambda e, ci_=ci_, val=val: e.memset(cst[:, ci_:ci_ + 1], val), w=[("cst", ci_)])
        cstr = [("cst", q) for q in range(4)]

        sp_load(b1, b1_in[:, l], "b1")
        sp_load(b2, moe_b2[l], "cm")
        sp_load(rw, router_w[l].rearrange("(c p) e -> p c e", p=128), "rw")
        sp_load(rb, router_b[l:l + 1, :], "rb")
        S.add('dve', lambda e: e.tensor_scalar_mul(out=b1s, in0=b1[:, :, 0:8], scalar1=1.702), r=["b1"], w=["b1s"])
        S.add('dve', lambda e: e.tensor_scalar_add(out=b1[:, :, 8:16], in0=b1[:, :, 8:16], scalar1=1.0),
              r=["b1"], w=["b1"])

        layernorm(l, 1, 24, h32=h32)

        for tt in range(8):
            ts_ = slice(tt * 128, (tt + 1) * 128)
            for c in range(NC_):
                S.add('pe', lambda e, c=c, ts_=ts_: e.matmul(ps(4)[:, 0:NE], lhsT=h32[:, c, ts_], rhs=rw[:, c, :],
                                                              start=(c == 0), stop=False),
                      r=[("h32", c), "rw"], w=[psr(4)])
            S.add('pe', lambda e: e.matmul(ps(4)[:, 0:NE], lhsT=ones_f[0:1, 0:128], rhs=rb, start=False, stop=True),
                  r=["ones_f", "rb"], w=[psr(4)])
            S.add('dve', lambda e: e.tensor_copy(out=lg, in_=ps(4)[:, 0:NE]), r=[psr(4)], w=["lg"])
            S.add('dve', lambda e: e.max(out=mx8, in_=lg), r=["lg"], w=["mx8"])
            S.add('dve', lambda e: e.tensor_scalar_mul(out=sm[:, 0:1], in0=mx8[:, 0:1], scalar1=-1.0),
                  r=["mx8"], w=["sm0"])
            S.add('act', lambda e: e.activation(out=ex, in_=lg, func=AF.Exp, bias=sm[:, 0:1], scale=1.0),
                  r=["lg", "sm0"], w=["ex"])
            S.add('dve', lambda e: e.scalar_tensor_tensor(out=ex, in0=lg, scalar=mx8[:, 3:4], in1=ex,
                                                          op0=ALU.is_ge, op1=ALU.mult),
                  r=["lg", "mx8", "ex"], w=["ex"])
            S.add('dve', lambda e: e.reduce_sum(out=sm[:, 1:2], in_=ex, axis=mybir.AxisListType.X),
                  r=["ex"], w=["sm1"])
            S.add('dve', lambda e: e.reciprocal(out=sm[:, 2:3], in_=sm[:, 1:2]), r=["sm1"], w=["sm2"])
            S.add('dve', lambda e: e.tensor_scalar_mul(out=ex, in0=ex, scalar1=sm[:, 2:3]), r=["ex", "sm2"], w=["ex"])
            S.add('pe', lambda e: e.transpose(out=ps(4)[0:32, 128:256], in_=ex, identity=ident_f),
                  r=["ex", "ident_f"], w=[psr(4)])
            S.add('dve', lambda e, ts_=ts_: e.tensor_copy(out=combT[:, ts_], in_=ps(4)[0:32, 128:256]),
                  r=[psr(4)], w=["combT"])

        for c in range(NC_):
            for th in range(2):
                cs = slice(th * 512, (th + 1) * 512)
                pb = 4 + (c * 2 + th) % 2
                S.add('pe', lambda e, c=c, cs=cs, pb=pb: e.matmul(ps(pb), lhsT=b2[:, c * 128:(c + 1) * 128],
                                                                   rhs=combT[:, cs], start=True, stop=True),
                      r=["cm", "combT"], w=[psr(pb)])
                S.add('act', lambda e, c=c, cs=cs, pb=pb: e.copy(out=R[:, c, cs], in_=ps(pb)),
                      r=[psr(pb)], w=[("R", c, th)])

        def load_w1(e_):
            nm = ("W1", e_ % 2)
            S.add('pool', lambda e: e.dma_start(out=W1[e_ % 2],
                                                in_=moe_w1[l, e_].rearrange("(c p) f -> p c f", p=128)),
                  w=[nm] + ([("h32", c) for c in range(NC_)] if e_ % 2 == 1 else []), dma=nm)

        def load_w2(e_):
            S.add('pool', lambda e: e.dma_start(out=W2, in_=moe_w2[l, e_].rearrange("(c p) f -> p c f", p=128)),
                  w=["W2"], dma="W2")

        def cm_for(e_):
            S.add('dve', lambda e: e.tensor_scalar_mul(out=cm, in0=combT, scalar1=ident_f[0:32, e_:e_ + 1]),
                  r=["combT", "ident_f"], w=["cm"])

        def cb_half(e_, th):
            cs = slice(th * 512, (th + 1) * 512)
            S.add('pe', lambda e: e.matmul(ps(6 + th), lhsT=ones_f[0:32, 0:128], rhs=cm[:, cs], start=True, stop=True),
                  r=["ones_f", "cm"], w=[psr(6 + th)])
            S.add('act', lambda e: e.copy(out=CB[:, cs], in_=ps(6 + th)), r=[psr(6 + th)], w=[("CB", th)])

        def h1_step(e_, i, th):
            if SUB < 2:
                return
            cs = slice(th * 512, (th + 1) * 512)
            w1 = W1[e_ % 2]
            nm = ("W1", e_ % 2)
            sb = (i + th) % 2
            pg, pu = 2 * sb, 2 * sb + 1
            for c in range(NC_):
                S.add('pe', lambda e, c=c: e.matmul(ps(pg), lhsT=w1[:, c, i * 128:(i + 1) * 128], rhs=hb[:, c, cs],
                                                    start=(c == 0), stop=(c == NC_ - 1)),
                      r=[nm, ("hb", c)], w=[psr(pg)])
            for c in range(NC_):
                S.add('pe', lambda e, c=c: e.matmul(ps(pu), lhsT=w1[:, c, 1024 + i * 128:1024 + (i + 1) * 128],
                                                    rhs=hb[:, c, cs], start=(c == 0), stop=(c == NC_ - 1)),
                      r=[nm, ("hb", c)], w=[psr(pu)])
            sg = SIG[sb]
            if SUB < 3:
                return
            S.add('act', lambda e: e.activation(out=GS, in_=ps(pg), func=AF.Identity, bias=b1[:, e_, i:i + 1], scale=1.0),
                  r=[psr(pg), "b1"], w=["RS"])
            S.add('act', lambda e: e.activation(out=US, in_=ps(pu), func=AF.Identity, bias=b1[:, e_, 8 + i:9 + i], scale=1.0),
                  r=[psr(pu), "b1"], w=["US"])
            if SUB < 4:
                return
            S.add('dve', lambda e: e.tensor_scalar_min(out=GS, in0=GS, scalar1=7.0), r=["RS"], w=["RS"])
            S.add('act', lambda e: e.activation(out=sg, in_=GS, func=AF.Sigmoid, scale=1.702), r=["RS"], w=[("SQ", sb)])
            S.add('dve', lambda e: e.tensor_scalar(out=US, in0=US, scalar1=8.0, scalar2=-6.0, op0=ALU.min, op1=ALU.max),
                  r=["US"], w=["US"])
            S.add('dve', lambda e: e.tensor_tensor(out=GS, in0=GS, in1=sg, op=ALU.mult), r=["RS", ("SQ", sb)], w=["RS"])
            S.add('dve', lambda e: e.tensor_tensor(out=US, in0=US, in1=GS, op=ALU.mult), r=["US", "RS"], w=["US"])
            S.add('dve', lambda e: e.tensor_tensor(out=aT[:, i, cs], in0=US, in1=CB[:, cs], op=ALU.mult),
                  r=["US", ("CB", th)], w=[("aT", i, th)])

        def y_step(e_, c, th):
            if SUB < 5:
                return
            cs = slice(th * 512, (th + 1) * 512)
            pb = 4 + (c + th) % 2
            for k in range(NC_):
                S.add('pe', lambda e, k=k: e.matmul(ps(pb), lhsT=W2[:, k, c * 128:(c + 1) * 128], rhs=aT[:, k, cs],
                                                    start=(k == 0), stop=(k == NC_ - 1)),
                      r=["W2", ("aT", k, th)], w=[psr(pb)])
            S.add('dve', lambda e: e.tensor_tensor(out=R[:, c, cs], in0=R[:, c, cs], in1=ps(pb), op=ALU.add),
                  r=[psr(pb), ("R", c, th)], w=[("R", c, th)])

        NEX = min(NE, NED) if STAGE >= 4 else 0
        if NEX > 0:
            load_w1(0)
        if NEX > 1:
            load_w1(1)
        PULL = 2
        for e_ in range(NEX):
            if e_ == 0:
                load_w2(0)
                cm_for(0)
                cb_half(0, 0)
                cb_half(0, 1)
                for i in range(NC_):
                    h1_step(0, i, 0)
            if e_ + 1 < NEX:
                cm_for(e_ + 1)
            for i in range(NC_):
                h1_step(e_, i, 1)
                y_step(e_, i, 0)
            if e_ + 2 < NEX:
                load_w1(e_ + 2)
            if e_ + 1 < NEX:
                cb_half(e_ + 1, 0)
                for i in range(PULL):
                    h1_step(e_ + 1, i, 0)
            for c in range(NC_):
                y_step(e_, c, 1)
            if e_ + 1 < NEX:
                cb_half(e_ + 1, 1)
                load_w2(e_ + 1)
                for i in range(PULL, NC_):
                    h1_step(e_ + 1, i, 0)

        for c in range(NC_):
            for th in range(2):
                cs = slice(th * 512, (th + 1) * 512)
                S.add('dve', lambda e, c=c, cs=cs: e.scalar_tensor_tensor(
                    out=R[:, c, cs], in0=R[:, c, cs], scalar=modT[:, 40 + c:41 + c], in1=xT[:, c, cs],
                    op0=ALU.mult, op1=ALU.add), r=["modT", ("xT", c), ("R", c, th)], w=[("R", c, th)])
        A.release()

    for li, (l, kind, j) in enumerate(layers):
        if STAGE >= 1:
            adaln(l)
            modulate(xT, 0)
        if STAGE >= 2:
            if kind == 0:
                conv_mixer(l, j)
            elif kind == 1:
                attention(l, j, False)
            else:
                attention(l, j, True)
        if STAGE >= 3:
            moe(l, li == len(layers) - 1)
        if STAGE >= 5:
            layernorm(l, 2, None)

    for c in range(NC_):
        nm = ("yst", c)
        i = S.add('sp', lambda e, c=c: e.dma_start(out=yT_out[c * 128:(c + 1) * 128, :], in_=xT[:, c, :]),
                  r=[("xT", c)] + [("xTh", c, th) for th in range(2)], w=[nm], dma=nm)
        final_waits.append(i)

    S.emit(nc, final_waits)
    es.close()
    return nc


def _fm(v):
    v = np.asarray(v, np.float32)
    lead = v.shape[:-1]
    return np.ascontiguousarray(np.moveaxis(v.reshape(lead + (NC_, 128)), -1, 0))


def _prep_common(inp):
    L = DEPTH
    com = {}
    com["w_mod"] = np.ascontiguousarray(inp["w_mod"][:LD], np.float32)
    com["bmod"] = np.ascontiguousarray(
        np.asarray(inp["b_mod"], np.float32).reshape(L, 48, 128).transpose(2, 0, 1))
    lnp = np.stack([inp["ln1_g"], inp["ln1_b"], inp["ln2_g"], inp["ln2_b"]], axis=1)
    com["lnp"] = np.ascontiguousarray(np.asarray(lnp, np.float32).reshape(L, 4, NC_, 128).transpose(3, 0, 1, 2))
    com["router_w"] = np.ascontiguousarray(inp["router_w"], np.float32)
    com["router_b"] = np.ascontiguousarray(inp["router_b"], np.float32)
    com["moe_w1"] = np.ascontiguousarray(inp["moe_w1"][:LD, :NED], np.float32)
    com["moe_w2"] = np.ascontiguousarray(inp["moe_w2"][:LD, :NED], np.float32)
    com["b1"] = np.ascontiguousarray(
        np.asarray(inp["moe_b1"], np.float32).reshape(L, NE, 16, 128).transpose(3, 0, 1, 2))
    com["moe_b2"] = np.ascontiguousarray(inp["moe_b2"], np.float32)
    com["conv_w_in"] = np.ascontiguousarray(inp["conv_w_in"], np.float32)
    com["conv_w_out"] = np.ascontiguousarray(inp["conv_w_out"], np.float32)
    cw = np.asarray(inp["conv_w"], np.float32)
    cbias = np.asarray(inp["conv_b"], np.float32)
    cp = np.concatenate([cw, cbias[:, None, :]], axis=1)
    com["convp"] = np.ascontiguousarray(cp.reshape(2, 4, NC_, 128).transpose(3, 0, 1, 2))
    com["ident_f"] = np.eye(128, dtype=np.float32)
    com["ident_b"] = np.eye(128, dtype=np.float32).astype(ml_dtypes.bfloat16)
    com["onesD"] = np.full((128, 128), 1.0 / D, np.float32)
    com["ones_f"] = np.ones((128, 128), np.float32)
    com["attn_w_qkv"] = np.ascontiguousarray(inp["attn_w_qkv"], np.float32)
    com["attn_w_o"] = np.ascontiguousarray(inp["attn_w_o"], np.float32)
    qn = np.asarray(inp["attn_q_norm"], np.float32)[0]
    kn = np.asarray(inp["attn_k_norm"], np.float32)[0]
    com["qkn"] = np.ascontiguousarray(np.stack([np.tile(qn, 2), np.tile(kn, 2)], axis=1))
    com["na_w_qkv"] = np.ascontiguousarray(inp["na_w_qkv"], np.float32)
    com["na_w_o"] = np.ascontiguousarray(inp["na_w_o"], np.float32)
    P = np.zeros((128, 128), np.float32)
    for hh in range(2):
        for half in range(2):
            b = hh * 64 + half * 32
            for i in range(16):
                P[b + 16 + i, b + i] = -1.0
                P[b + i, b + 16 + i] = 1.0
    com["perm"] = P
    blk = np.zeros((128, 128), np.float32)
    blk[:64, :64] = 1.0 / 64
    blk[64:, 64:] = 1.0 / 64
    com["blk"] = blk
    sw = np.zeros((128, 128), np.float32)
    for m in range(128):
        sw[(m + 64) % 128, m] = 1.0
    com["swp"] = sw
    return com


def _prep_core(inp, ci):
    d = {}
    if ci < 2:
        tok = np.asarray(inp["x_sample"], np.float32)[ci]
        cond = np.asarray(inp["c"], np.float32)[ci]
        seqlen = 1024
    else:
        s0 = 4 * (ci - 2)
        tok = np.asarray(inp["x_prompt"], np.float32)[s0:s0 + 4].reshape(T, D)
        cond = np.asarray(inp["c_ctx"], np.float32)
        seqlen = 256
    d["xT_in"] = np.ascontiguousarray(tok.T)
    d["cond"] = np.ascontiguousarray(cond.reshape(NC_, 128).T)
    t = np.arange(T)
    ml = (t % seqlen != 0).astype(np.float32)
    mr = (t % seqlen != seqlen - 1).astype(np.float32)
    d["cmask"] = np.ascontiguousarray(np.broadcast_to(np.stack([ml, mr])[None], (128, 2, T)))
    bf = ml_dtypes.bfloat16
    k = np.arange(1280)
    q = np.arange(T)
    tk = k - 256
    if ci < 2:
        inv = (np.float32(10000.0) ** (-np.arange(16, dtype=np.float32) / np.float32(16))).astype(np.float32)
        row = (t // 64).astype(np.float32)
        col = (t % 64).astype(np.float32)
        cos = np.zeros((128, T), np.float32)
        sin = np.zeros((128, T), np.float32)
        for p in range(128):
            dd = p % 64
            pos = row if dd < 32 else col
            ang = (pos * inv[(dd % 32) % 16]).astype(np.float32)
            cos[p] = np.cos(ang)
            sin[p] = np.sin(ang)
        d["rope"] = np.ascontiguousarray(np.stack([cos, sin], axis=1))
        ck = np.asarray(inp["cache_k_attn"], np.float32)[ci, 0]
        d["ckT_attn"] = np.ascontiguousarray(np.tile(ck.transpose(2, 1, 0), (2, 1, 1)))
        d["cv_attn"] = np.ascontiguousarray(np.asarray(inp["cache_v_attn"], np.float32)[ci, 0].reshape(256, 256))
        d["ckT_na"] = np.ascontiguousarray(np.asarray(inp["cache_k_na"], np.float32)[ci, 0].reshape(256, D).T)
        d["cv_na"] = np.ascontiguousarray(np.asarray(inp["cache_v_na"], np.float32)[ci, 0].reshape(256, D))
        am = np.zeros((1280, T), np.float32)
        kr, kc = tk // 64, tk % 64
        qr, qc = q // 64, q % 64
        rs = np.clip(qr - 4, 0, 8)
        cs = np.clip(qc - 8, 0, 48)
        ok = ((kr[:, None] >= rs[None]) & (kr[:, None] < rs[None] + 8) &
              (kc[:, None] >= cs[None]) & (kc[:, None] < cs[None] + 16))
        nm = np.where(ok, 0.0, NEG).astype(np.float32)
        nm[:256] = 0.0
        rpb = np.asarray(inp["na_rpb"], np.float32)[0]
        cb2 = np.zeros((16, 2, 64, 16, 64), np.float32)
        kcs = np.arange(64)
        co = kcs[:, None] - kcs[None, :] + 15
        cok = (co >= 0) & (co <= 30)
        coc = np.clip(co, 0, 30)
        for dr in range(2):
            for jj in range(16):
                ro = 14 - jj + dr
                if 0 <= ro <= 14:
                    cb2[:, dr, :, jj, :] = np.where(cok[None], rpb[:, ro][:, coc], 0.0)
        d["cb2"] = np.ascontiguousarray(cb2.reshape(16, 128, 1024)).astype(bf)
    else:
        d["rope"] = np.ascontiguousarray(np.stack([np.ones((128, T), np.float32), np.zeros((128, T), np.float32)], axis=1))
        d["ckT_attn"] = np.zeros((128, 4, 256), np.float32)
        d["cv_attn"] = np.zeros((256, 256), np.float32)
        d["ckT_na"] = np.zeros((D, 256), np.float32)
        d["cv_na"] = np.zeros((256, D), np.float32)
        ok = (tk[:, None] // 256) == (q[None] // 256)
        am = np.where(ok, 0.0, NEG).astype(np.float32)
        am[:256] = NEG
        nm = am
        d["cb2"] = np.zeros((16, 128, 1024), bf)
    d["amask"] = np.ascontiguousarray(am.reshape(10, 128, T).transpose(1, 0, 2)).astype(bf)
    d["nmask"] = np.ascontiguousarray(nm.reshape(10, 128, T).transpose(1, 0, 2)).astype(bf)
    return d


_PROG_CACHE = {}


def _get_prog(layers):
    key = tuple(layers)
    if key not in _PROG_CACHE:
        _PROG_CACHE[key] = build_program(list(layers))
    return _PROG_CACHE[key]


def kernel(**inputs):
    layers = [(l, l % 3, l // 3) for l in range(DEPTH)]
    nc = _get_prog(layers)
    com = _prep_common(inputs)
    in_maps = []
    for ci in range(8):
        d = dict(com)
        d.update(_prep_core(inputs, min(ci, NCORES - 1)))
        in_maps.append(d)
    res = run_bass_kernel_spmd(nc, in_maps, core_ids=list(range(8)))
    R_ = res.results
    y_s = np.stack([np.asarray(R_[ci]["yT_out"]).T for ci in range(2)]).astype(np.float32)
    y_p = np.concatenate([np.asarray(R_[ci]["yT_out"]).T.reshape(4, 256, D) for ci in range(2, 6)]).astype(np.float32)
    nk_a = np.concatenate([np.asarray(R_[ci]["kT_attn_out"]).T.reshape(4, 256, 4, 64) for ci in range(2, 6)])
    nv_a = np.concatenate([np.asarray(R_[ci]["v_attn_out"]).reshape(4, 256, 4, 64) for ci in range(2, 6)])
    nk_n = np.concatenate([np.asarray(R_[ci]["kT_na_out"]).T.reshape(4, 256, 16, 64) for ci in range(2, 6)])
    nv_n = np.concatenate([np.asarray(R_[ci]["v_na_out"]).reshape(4, 256, 16, 64) for ci in range(2, 6)])
    f = lambda a: np.ascontiguousarray(a[:, None], dtype=np.float32)
    return (np.ascontiguousarray(y_p), np.ascontiguousarray(y_s), f(nk_a), f(nv_a), f(nk_n), f(nv_n))
```

```python
import numpy as np
import ml_dtypes
from contextlib import ExitStack
import concourse.bass as bass
import concourse.mybir as mybir
from concourse.bass_utils import run_bass_kernel_spmd

F32 = mybir.dt.float32
BF16 = mybir.dt.bfloat16
ALU = mybir.AluOpType
AF = mybir.ActivationFunctionType

D = 1024
T = 1024
NC_ = 8
NCORES = 6
DEPTH = 4
NE = 32
ALPHA = (2 * DEPTH) ** 0.25
BETA = (8 * DEPTH) ** -0.25
LN_EPS = 1e-5
RMS_EPS = 1e-6
NEG = -30000.0
S7 = float(1.0 / (1.0 + np.exp(-1.702 * 7.0)))
import os
CAP = int(os.environ.get('KCAP', '4000'))
STAGE = int(os.environ.get('KSTAGE', '9'))
LD = int(os.environ.get('KLD', '4'))
NED = int(os.environ.get('KNED', '32'))
SUB = int(os.environ.get('KSUB', '9'))
KDV = int(os.environ.get('KDV', '9'))


class Sched:
    def __init__(self):
        self.ops = []
        self.lastw = {}
        self.rd_eng = {}
        self.rd_dma = {}
        self.seen = set()
        self.seen_order = []
        self.scopes = []
        self.fence = set()
        self.last_eng = {}
        self.last_dma = {}

    def mark(self):
        self.scopes.append(len(self.seen_order))

    def release(self):
        n = self.scopes.pop()
        for x in self.seen_order[n:]:
            self.seen.discard(x)
        del self.seen_order[n:]
        self.fence = set(self.last_eng.values()) | set(self.last_dma.values())

    def add(self, eng, fn, r=(), w=(), dma=None):
        deps = set()
        for x in list(r) + list(w):
            if x not in self.seen:
                self.seen.add(x)
                self.seen_order.append(x)
                deps |= self.fence
        for x in r:
            if x in self.lastw:
                deps.add(self.lastw[x])
        for x in w:
            if x in self.lastw:
                deps.add(self.lastw[x])
            deps.update(self.rd_eng.get(x, {}).values())
            deps.update(self.rd_dma.get(x, ()))
        i = len(self.ops)
        self.ops.append(dict(eng=eng, fn=fn, deps=deps, dma=dma))
        if dma is None:
            self.last_eng[eng] = i
        else:
            self.last_dma[dma] = i
        for x in r:
            if dma is None:
                self.rd_eng.setdefault(x, {})[eng] = i
            else:
                self.rd_dma.setdefault(x, []).append(i)
        for x in w:
            self.lastw[x] = i
            self.rd_eng[x] = {}
            self.rd_dma[x] = []
        return i

    def emit(self, nc, final_waits):
        ops = self.ops
        needed = set()
        for op in ops:
            for d_ in op['deps']:
                if op['eng'] == 'pe' and op['dma'] is None and ops[d_]['eng'] == 'pe' and ops[d_]['dma'] is None:
                    continue
                needed.add(d_)
        for i in final_waits:
            needed.add(i)
        engs = ['pe', 'act', 'dve', 'pool', 'sp']
        seq = {e: 0 for e in engs}
        dcount = {}
        ev = {}
        for i, op in enumerate(ops):
            if op['dma'] is not None:
                n = dcount.get(op['dma'], 0)
                dcount[op['dma']] = n + 1
                ev[i] = (('d', op['dma']), 16 * (n + 1))
            elif i in needed:
                e = op['eng']
                s = seq[e]
                seq[e] = s + 1
                ev[i] = (('e', e, s // CAP), s % CAP + 1)
        semkeys = []
        for i in sorted(ev):
            if ev[i][0] not in semkeys:
                semkeys.append(ev[i][0])
        with ExitStack() as es:
            sems = {}
            for k in semkeys:
                sems[k] = es.enter_context(nc.semaphore("s_" + "_".join(str(x) for x in k[1:]).replace(" ", "")
                                                        .replace("(", "").replace(")", "").replace(",", "_").replace("'", "")))
            block = es.enter_context(nc.Block())
            per = {e: [i for i, op in enumerate(ops) if op['eng'] == e] for e in engs}

            def run(e, eng):
                waited = {}
                for i in per[e]:
                    op = ops[i]
                    for d in sorted(op['deps']):
                        if d not in ev:
                            continue
                        if ops[d]['eng'] == 'pe' and e == 'pe' and ops[d]['dma'] is None:
                            continue
                        k, v = ev[d]
                        if waited.get(k, 0) < v:
                            eng.wait_ge(sems[k], v)
                            waited[k] = v
                    ins = op['fn'](eng)
                    if i in ev:
                        k, v = ev[i]
                        ins.then_inc(sems[k], 16 if k[0] == 'd' else 1)
                if e == 'sp':
                    for i in final_waits:
                        k, v = ev[i]
                        eng.wait_ge(sems[k], v)

            block.tensor(lambda eng: run('pe', eng))
            block.scalar(lambda eng: run('act', eng))
            block.vector(lambda eng: run('dve', eng))
            block.gpsimd(lambda eng: run('pool', eng))
            block.sync(lambda eng: run('sp', eng))


class Arena:
    def __init__(self, ap, words):
        self.ap = ap
        self.words = words
        self.top = 0
        self.marks = []
        self.sched = None

    def alloc(self, words):
        words = (words + 7) // 8 * 8
        o = self.top
        self.top += words
        assert self.top <= self.words, (self.top, self.words)
        return o

    def f32(self, shape):
        n = int(np.prod(shape[1:]))
        o = self.alloc(n)
        v = self.ap[0:shape[0], o:o + n]
        if len(shape) == 3:
            v = v.rearrange("p (a b) -> p a b", a=shape[1])
        elif len(shape) == 4:
            v = v.rearrange("p (a b c) -> p a b c", a=shape[1], b=shape[2])
        return v

    def bf16(self, shape):
        n = int(np.prod(shape[1:]))
        o = self.alloc((n + 1) // 2)
        v = self.ap[0:shape[0], o:o + (n + 1) // 2].bitcast(BF16)[:, 0:n]
        if len(shape) == 3:
            v = v.rearrange("p (a b) -> p a b", a=shape[1])
        elif len(shape) == 4:
            v = v.rearrange("p (a b c) -> p a b c", a=shape[1], b=shape[2])
        return v

    def _shape(self, v, shape):
        if len(shape) == 3:
            v = v.rearrange("p (a b) -> p a b", a=shape[1])
        elif len(shape) == 4:
            v = v.rearrange("p (a b c) -> p a b c", a=shape[1], b=shape[2])
        return v

    def f32_at(self, o, shape):
        n = int(np.prod(shape[1:]))
        return self._shape(self.ap[0:shape[0], o:o + n], shape)

    def bf16_at(self, o, shape):
        n = int(np.prod(shape[1:]))
        return self._shape(self.ap[0:shape[0], o:o + (n + 1) // 2].bitcast(BF16)[:, 0:n], shape)

    def mark(self):
        self.marks.append(self.top)
        if self.sched is not None:
            self.sched.mark()

    def release(self):
        self.top = self.marks.pop()
        if self.sched is not None:
            self.sched.release()


def build_program(layers, debug_out=False):
    nc = bass.Bass("TRN2", target_bir_lowering=False)
    S = Sched()
    L = DEPTH

    def din(name, shape, dt=F32):
        return nc.dram_tensor(name, list(shape), dt, kind="ExternalInput").ap()

    def dout(name, shape, dt=F32):
        return nc.dram_tensor(name, list(shape), dt, kind="ExternalOutput").ap()

    xT_in = din("xT_in", [D, T])
    cond_in = din("cond", [128, NC_])
    w_mod = din("w_mod", [LD, D, 6 * D])
    bmod_in = din("bmod", [128, L, 48])
    lnp_in = din("lnp", [128, L, 4, NC_])
    router_w = din("router_w", [L, D, NE])
    router_b = din("router_b", [L, NE])
    moe_w1 = din("moe_w1", [LD, NED, D, 2 * D])
    moe_w2 = din("moe_w2", [LD, NED, D, D])
    b1_in = din("b1", [128, L, NE, 16])
    moe_b2 = din("moe_b2", [L, NE, D])
    conv_w_in = din("conv_w_in", [2, D, 3 * D])
    conv_w_out = din("conv_w_out", [2, D, D])
    convp_in = din("convp", [128, 2, 4, NC_])
    cmask_in = din("cmask", [128, 2, T])
    ident_f_in = din("ident_f", [128, 128])
    ident_b_in = din("ident_b", [128, 128], BF16)
    onesD_in = din("onesD", [128, 128])
    ones_in = din("ones_f", [128, 128])
    attn_w_qkv = din("attn_w_qkv", [1, D, 1536])
    attn_w_o = din("attn_w_o", [1, D, D])
    qkn_in = din("qkn", [128, 2])
    rope_in = din("rope", [128, 2, T])
    perm_in = din("perm", [128, 128])
    blk_in = din("blk", [128, 128])
    ckT_attn = din("ckT_attn", [128, 4, 256])
    swp_in = din("swp", [128, 128])
    cv_attn = din("cv_attn", [256, 256])
    amask_in = din("amask", [128, 10, T], BF16)
    na_w_qkv = din("na_w_qkv", [1, D, 3 * D])
    na_w_o = din("na_w_o", [1, D, D])
    ckT_na = din("ckT_na", [D, 256])
    cv_na = din("cv_na", [256, D])
    nmask_in = din("nmask", [128, 10, T], BF16)
    cb2_in = din("cb2", [16, 128, 16 * 64], BF16)

    yT_out = dout("yT_out", [D, T])
    kT_attn_out = dout("kT_attn_out", [256, T])
    v_attn_out = dout("v_attn_out", [T, 256])
    kT_na_out = dout("kT_na_out", [D, T])
    v_na_out = dout("v_na_out", [T, D])

    AW = 53100
    es = ExitStack()
    arena_t = es.enter_context(nc.sbuf_tensor("arena", [128, AW], F32))
    A = Arena(arena_t, AW)
    A.sched = S
    PS = [es.enter_context(nc.psum_tensor("ps%d" % i, [128, 512], F32)) for i in range(8)]

    def ps(i):
        return PS[i][:, :]

    def psr(i):
        return ("ps", i)

    final_waits = []

    xT = A.f32([128, NC_, T])
    hb = A.bf16([128, NC_, T])
    R = A.f32([128, NC_, T])
    ident_f = A.f32([128, 128])
    onesD = A.f32([128, 128])
    ones_f = A.f32([128, 128])
    ident_b = A.bf16([128, 128])
    condT = A.f32([128, NC_])
    scT = A.bf16([128, NC_])
    bmod = A.f32([128, L, 48])
    lnp = A.f32([128, L, 4, NC_])
    convp = A.f32([128, 2, 4, NC_])
    modT = A.f32([128, 48])
    SQ = [A.f32([128, 512]) for _ in range(2)]
    RS = A.f32([128, 512])

    def sp_load(dst, src, name, eng='sp', res=None):
        S.add(eng, lambda e, d=dst, s=src: e.dma_start(out=d, in_=s), w=(res or [name]), dma=name)

    sp_load(xT, xT_in.rearrange("(c p) t -> p c t", p=128), "xT", res=[("xT", c) for c in range(NC_)])
    sp_load(condT, cond_in, "condT")
    sp_load(ident_f, ident_f_in, "ident_f")
    sp_load(onesD, onesD_in, "onesD")
    sp_load(ones_f, ones_in, "ones_f")
    sp_load(ident_b, ident_b_in, "ident_b")
    sp_load(bmod, bmod_in, "bmod")
    sp_load(lnp, lnp_in, "lnp")
    sp_load(convp, convp_in, "convp")
    S.add('act', lambda e: e.activation(out=scT, in_=condT, func=AF.Silu), r=["condT"], w=["scT"])

    def adaln(l):
        A.mark()
        Wm = [A.bf16([128, NC_, 1024]) for _ in range(2)]
        for g in range(6):
            wb = Wm[g % 2]
            nm = ("Wm", g % 2)
            S.add('pool', lambda e, wb=wb, g=g: e.dma_start(
                out=wb, in_=w_mod[l, :, g * 1024:(g + 1) * 1024].rearrange("(c p) f -> p c f", p=128)),
                w=[nm], dma=nm)
            for kk in range(8):
                k = g * 8 + kk
                for c in range(NC_):
                    S.add('pe', lambda e, wb=wb, kk=kk, c=c, k=k: e.matmul(
                        ps(7)[:, k:k + 1], lhsT=wb[:, c, kk * 128:(kk + 1) * 128], rhs=scT[:, c:c + 1],
                        start=(c == 0), stop=(c == NC_ - 1)),
                        r=[nm, "scT"], w=[psr(7)])
        S.add('dve', lambda e: e.tensor_tensor(out=modT, in0=ps(7)[:, 0:48], in1=bmod[:, l, :], op=ALU.add),
              r=[psr(7), "bmod"], w=["modT"])
        for lo in (8, 32):
            S.add('dve', lambda e, lo=lo: e.tensor_scalar_add(out=modT[:, lo:lo + 8], in0=modT[:, lo:lo + 8], scalar1=1.0),
                  r=["modT"], w=["modT"])
        for lo in (16, 40):
            S.add('dve', lambda e, lo=lo: e.tensor_scalar_mul(out=modT[:, lo:lo + 8], in0=modT[:, lo:lo + 8],
                                                                scalar1=1.0 / ALPHA), r=["modT"], w=["modT"])
        A.release()

    def modulate(src, off):
        for c in range(NC_):
            S.add('dve', lambda e, c=c: e.tensor_scalar(
                out=hb[:, c, :], in0=src[:, c, :], scalar1=modT[:, off + 8 + c:off + 9 + c],
                scalar2=modT[:, off + c:off + c + 1], op0=ALU.mult, op1=ALU.add),
                r=["modT", ("xT", c)], w=[("hb", c)])

    def layernorm(l, which, mod_off, h32=None):
        gi, bi = (0, 1) if which == 1 else (2, 3)
        eps = LN_EPS / (ALPHA * ALPHA)
        for th in range(2):
            cs = slice(th * 512, (th + 1) * 512)
            pm, pv = 5, 6
            for c in range(NC_):
                S.add('pe', lambda e, c=c, cs=cs: e.matmul(ps(pm), lhsT=onesD, rhs=R[:, c, cs],
                                                           start=(c == 0), stop=(c == NC_ - 1)),
                      r=["onesD", ("R", c, th)], w=[psr(pm)])
            for c in range(NC_):
                S.add('dve', lambda e, c=c, cs=cs: e.tensor_tensor(out=R[:, c, cs], in0=R[:, c, cs], in1=ps(pm),
                                                                   op=ALU.subtract),
                      r=[psr(pm), ("R", c, th)], w=[("R", c, th)])
            for c in range(NC_):
                sq = SQ[c % 2]
                S.add('act', lambda e, c=c, cs=cs, sq=sq: e.activation(out=sq, in_=R[:, c, cs], func=AF.Square),
                      r=[("R", c, th)], w=[("SQ", c % 2)])
                S.add('pe', lambda e, c=c, sq=sq: e.matmul(ps(pv), lhsT=onesD, rhs=sq,
                                                           start=(c == 0), stop=(c == NC_ - 1)),
                      r=["onesD", ("SQ", c % 2)], w=[psr(pv)])
            S.add('dve', lambda e: e.tensor_scalar_add(out=RS, in0=ps(pv), scalar1=eps), r=[psr(pv)], w=["RS"])
            S.add('act', lambda e: e.activation(out=RS, in_=RS, func=AF.Sqrt), r=["RS"], w=["RS"])
            S.add('dve', lambda e: e.reciprocal(out=RS, in_=RS), r=["RS"], w=["RS"])
            for c in range(NC_):
                S.add('dve', lambda e, c=c, cs=cs: e.tensor_tensor(out=R[:, c, cs], in0=R[:, c, cs], in1=RS, op=ALU.mult),
                      r=["RS", ("R", c, th)], w=[("R", c, th)])
                S.add('dve', lambda e, c=c, cs=cs: e.tensor_scalar(
                    out=xT[:, c, cs], in0=R[:, c, cs], scalar1=lnp[:, l, gi, c:c + 1], scalar2=lnp[:, l, bi, c:c + 1],
                    op0=ALU.mult, op1=ALU.add), r=["lnp", ("R", c, th)], w=[("xT", c), ("xTh", c, th)])
                if mod_off is not None:
                    S.add('dve', lambda e, c=c, cs=cs: e.tensor_scalar(
                        out=hb[:, c, cs], in0=xT[:, c, cs], scalar1=modT[:, mod_off + 8 + c:mod_off + 9 + c],
                        scalar2=modT[:, mod_off + c:mod_off + c + 1], op0=ALU.mult, op1=ALU.add),
                        r=["modT", ("xTh", c, th)], w=[("hb", c)])
                    if h32 is not None:
                        S.add('pool', lambda e, c=c, cs=cs: e.tensor_scalar(
                            out=h32[:, c, cs], in0=xT[:, c, cs], scalar1=modT[:, mod_off + 8 + c:mod_off + 9 + c],
                            scalar2=modT[:, mod_off + c:mod_off + c + 1], op0=ALU.mult, op1=ALU.add),
                            r=["modT", ("xTh", c, th)], w=[("h32", c), ("W1", 1)])

    def residual_from_psum(c, th, pbank, gate_off):
        cs = slice(th * 512, (th + 1) * 512)
        S.add('dve', lambda e: e.scalar_tensor_tensor(
            out=R[:, c, cs], in0=ps(pbank), scalar=modT[:, gate_off + c:gate_off + c + 1], in1=xT[:, c, cs],
            op0=ALU.mult, op1=ALU.add), r=[psr(pbank), "modT", ("xT", c)], w=[("R", c, th)])

    def conv_mixer(l, j):
        A.mark()
        Wi = [A.bf16([128, NC_, 1024]) for _ in range(2)]
        Wo = A.bf16([128, NC_, 1024])
        U = A.f32([128, NC_, T])
        cmask = A.f32([128, 2, T])
        V = A.bf16([128, NC_, T])
        TMP = [A.f32([128, 512]) for _ in range(2)]
        sp_load(cmask, cmask_in, "cmask")
        for pi, part in enumerate((1, 2, 0)):
            wb = Wi[pi % 2]
            nm = ("Wi", pi % 2)
            S.add('pool', lambda e, wb=wb, part=part: e.dma_start(
                out=wb, in_=conv_w_in[j, :, part * 1024:(part + 1) * 1024].rearrange("(c p) f -> p c f", p=128)),
                w=[nm], dma=nm)
            if pi == 0:
                S.add('pool', lambda e: e.dma_start(out=Wo, in_=conv_w_out[j].rearrange("(c p) f -> p c f", p=128)),
                      w=["Wo"], dma="Wo")
            for fc in range(NC_):
                for th in range(2):
                    cs = slice(th * 512, (th + 1) * 512)
                    pb = (fc * 2 + th) % 4
                    for c in range(NC_):
                        S.add('pe', lambda e, wb=wb, fc=fc, c=c, cs=cs, pb=pb: e.matmul(
                            ps(pb), lhsT=wb[:, c, fc * 128:(fc + 1) * 128], rhs=hb[:, c, cs],
                            start=(c == 0), stop=(c == NC_ - 1)), r=[nm, ("hb", c)], w=[psr(pb)])
                    if part == 1:
                        S.add('act', lambda e, fc=fc, cs=cs, pb=pb: e.copy(out=U[:, fc, cs], in_=ps(pb)),
                              r=[psr(pb)], w=[("U", fc, th)])
                    elif part == 2:
                        S.add('dve', lambda e, fc=fc, cs=cs, pb=pb: e.tensor_tensor(
                            out=U[:, fc, cs], in0=U[:, fc, cs], in1=ps(pb), op=ALU.mult),
                            r=[psr(pb), ("U", fc, th)], w=[("U", fc, th)])
                    else:
                        tmp = TMP[th]
                        tn = ("TMP", th)
                        lo = th * 512
                        S.add('dve', lambda e, fc=fc, cs=cs, tmp=tmp: e.tensor_scalar(
                            out=tmp, in0=U[:, fc, cs], scalar1=convp[:, j, 1, fc:fc + 1], scalar2=convp[:, j, 3, fc:fc + 1],
                            op0=ALU.mult, op1=ALU.add), r=[("U", fc, 0), ("U", fc, 1), "convp"], w=[tn])
                        a0 = max(lo - 1, 0)
                        n0 = 512 - (1 if th == 0 else 0)
                        d0 = 1 if th == 0 else 0
                        A.mark()
                        S.add('pool', lambda e, fc=fc, a0=a0, n0=n0, lo=lo, d0=d0, th=th: e.tensor_tensor(
                            out=SQ[th][:, d0:d0 + n0], in0=U[:, fc, a0:a0 + n0], in1=cmask[:, 0, lo + d0:lo + d0 + n0],
                            op=ALU.mult), r=[("U", fc, 0), ("U", fc, 1), "cmask"], w=[("SQ", th)])
                        S.add('dve', lambda e, fc=fc, d0=d0, n0=n0, tmp=tmp, th=th: e.scalar_tensor_tensor(
                            out=tmp[:, d0:d0 + n0], in0=SQ[th][:, d0:d0 + n0], scalar=convp[:, j, 0, fc:fc + 1],
                            in1=tmp[:, d0:d0 + n0], op0=ALU.mult, op1=ALU.add), r=[("SQ", th), tn, "convp"], w=[tn])
                        n1 = 512 - (1 if th == 1 else 0)
                        S.add('pool', lambda e, fc=fc, lo=lo, n1=n1, th=th: e.tensor_tensor(
                            out=SQ[th][:, 0:n1], in0=U[:, fc, lo + 1:lo + 1 + n1], in1=cmask[:, 1, lo:lo + n1],
                            op=ALU.mult), r=[("U", fc, 0), ("U", fc, 1), "cmask", tn], w=[("SQ", th)])
                        S.add('dve', lambda e, fc=fc, n1=n1, tmp=tmp, th=th: e.scalar_tensor_tensor(
                            out=tmp[:, 0:n1], in0=SQ[th][:, 0:n1], scalar=convp[:, j, 2, fc:fc + 1],
                            in1=tmp[:, 0:n1], op0=ALU.mult, op1=ALU.add), r=[("SQ", th), tn, "convp"], w=[tn])
                        A.release()
                        S.add('dve', lambda e, fc=fc, cs=cs, tmp=tmp, pb=pb: e.tensor_tensor(
                            out=V[:, fc, cs], in0=tmp, in1=ps(pb), op=ALU.mult), r=[tn, psr(pb)], w=[("V", fc)])
        for fc in range(NC_):
            for th in range(2):
                cs = slice(th * 512, (th + 1) * 512)
                pb = (fc * 2 + th) % 4
                for c in range(NC_):
                    S.add('pe', lambda e, fc=fc, c=c, cs=cs, pb=pb: e.matmul(
                        ps(pb), lhsT=Wo[:, c, fc * 128:(fc + 1) * 128], rhs=V[:, c, cs],
                        start=(c == 0), stop=(c == NC_ - 1)), r=["Wo", ("V", c)], w=[psr(pb)])
                residual_from_psum(fc, th, pb, 16)
        A.release()


    def attention(l, j, is_na):
        nvh = 16 if is_na else 4
        NV = nvh * 64
        ng = 8 if is_na else 4
        wqkv = na_w_qkv if is_na else attn_w_qkv
        wo_d = na_w_o if is_na else attn_w_o
        mask_in = nmask_in if is_na else amask_in
        kT_out = kT_na_out if is_na else kT_attn_out
        v_out = v_na_out if is_na else v_attn_out
        A.mark()
        QT = A.bf16([128, NC_, T])
        KT = A.bf16([128, ng, 1280])
        VA = A.bf16([128, 10, nvh, 65])
        OTK = A.bf16([128, 8, D])
        REC = A.f32([128, 8])
        qk2 = A.f32([128, 2])
        A.mark()
        WP = [A.bf16([128, NC_, 512]) for _ in range(2)]
        KST = [A.f32([128, 512]) for _ in range(2)]
        VST = [A.f32([128, NV]) for _ in range(2)]
        if not is_na:
            ROPE = A.f32([128, 2, T])
            PERM = A.f32([128, 128])
            BLK = A.f32([128, 128])
            SWP = A.f32([128, 128])
            QN = A.f32([128, 512])
            T1 = A.f32([128, 512])
            T2 = A.f32([128, 512])
            sp_load(ROPE, rope_in, "ROPE")
            sp_load(PERM, perm_in, "PERM")
            sp_load(BLK, blk_in, "BLK")
            sp_load(SWP, swp_in, "SWP")
            sp_load(qk2, qkn_in, "qk2")
            S.add('dve', lambda e: e.tensor_scalar_mul(out=qk2[:, 0:1], in0=qk2[:, 0:1], scalar1=0.125),
                  r=["qk2"], w=["qk2"])
        S.add('dve', lambda e: e.memset(VA[:, :, :, 64:65], 1.0), w=[("VA1",)])
        if is_na:
            S.add('pool', lambda e: e.dma_start(out=KT[:, :, 0:256], in_=ckT_na.rearrange("(c p) t -> p c t", p=128)),
                  w=[("KTc",)], dma="KTc")
            for blk_ in range(2):
                S.add('pool', lambda e, blk_=blk_: e.dma_start(
                    out=VA[:, blk_, :, 0:64],
                    in_=cv_na[blk_ * 128:(blk_ + 1) * 128, :].rearrange("p (h d) -> p h d", d=64)),
                    w=[("VAc", blk_)], dma=("VAc", blk_))
        else:
            S.add('pool', lambda e: e.dma_start(out=KT[:, :, 0:256], in_=ckT_attn), w=[("KTc",)], dma="KTc")
            for blk_ in range(2):
                S.add('pool', lambda e, blk_=blk_: e.dma_start(
                    out=VA[:, blk_, :, 0:64],
                    in_=cv_attn[blk_ * 128:(blk_ + 1) * 128, :].rearrange("p (h d) -> p h d", d=64)),
                    w=[("VAc", blk_)], dma=("VAc", blk_))
        npieces = 6 if is_na else 3
        kst_i = 0
        for pi in range(npieces):
            wb = WP[pi % 2]
            nm = ("WP", pi % 2)
            S.add('pool', lambda e, wb=wb, pi=pi: e.dma_start(
                out=wb, in_=wqkv[j, :, pi * 512:(pi + 1) * 512].rearrange("(c p) f -> p c f", p=128)),
                w=[nm], dma=nm)
            if pi < 2:
                fm = [("q", pi * 4 + f, f) for f in range(4)]
                vcols = None
            elif is_na and pi < 4:
                fm = [("k", (pi - 2) * 4 + f, f) for f in range(4)]
                vcols = None
            elif is_na:
                fm = []
                vcols = (0, 512, (pi - 4) * 512)
            else:
                fm = [("k", f, f) for f in range(2)]
                vcols = (256, 256, 0)
            for (kind_, gc, fl) in fm:
                for th in range(2):
                    cs = slice(th * 512, (th + 1) * 512)
                    pb = (fl * 2 + th) % 4
                    for c in range(NC_):
                        S.add('pe', lambda e, wb=wb, fl=fl, c=c, cs=cs, pb=pb: e.matmul(
                            ps(pb), lhsT=wb[:, c, fl * 128:(fl + 1) * 128], rhs=hb[:, c, cs],
                            start=(c == 0), stop=(c == NC_ - 1)), r=[nm, ("hb", c)], w=[psr(pb)])
                    if is_na:
                        if kind_ == "q":
                            S.add('act', lambda e, gc=gc, cs=cs, pb=pb: e.mul(out=QT[:, gc, cs], in_=ps(pb), mul=0.125),
                                  r=[psr(pb)], w=[("QT", gc)])
                        else:
                            kst = KST[kst_i % 2]
                            kn = ("KST", kst_i % 2)
                            kst_i += 1
                            S.add('act', lambda e, kst=kst, pb=pb: e.copy(out=kst, in_=ps(pb)), r=[psr(pb)], w=[kn])
                            S.add('dve', lambda e, kst=kst, gc=gc, th=th: e.tensor_copy(
                                out=KT[:, gc, 256 + th * 512:256 + (th + 1) * 512], in_=kst), r=[kn], w=[("KT", gc)])
                            S.add('sp', lambda e, kst=kst, gc=gc, cs=cs: e.dma_start(
                                out=kT_out[gc * 128:(gc + 1) * 128, cs], in_=kst), r=[kn], w=[("kout", gc, th)], dma=kn)
                    else:
                        x = th
                        sq = SQ[x]
                        S.add('act', lambda e, sq=sq, pb=pb: e.activation(out=sq, in_=ps(pb), func=AF.Square),
                              r=[psr(pb)], w=[("SQ", x)])
                        S.add('pe', lambda e, sq=sq: e.matmul(ps(4), lhsT=BLK, rhs=sq, start=True, stop=True),
                              r=["BLK", ("SQ", x)], w=[psr(4)])
                        S.add('dve', lambda e: e.tensor_scalar_add(out=RS, in0=ps(4), scalar1=RMS_EPS), r=[psr(4)], w=["RS"])
                        S.add('act', lambda e: e.activation(out=RS, in_=RS, func=AF.Sqrt), r=["RS"], w=["RS"])
                        S.add('dve', lambda e: e.reciprocal(out=RS, in_=RS), r=["RS"], w=["RS"])
                        gcol = 0 if kind_ == "q" else 1
                        S.add('dve', lambda e, pb=pb, gcol=gcol: e.scalar_tensor_tensor(
                            out=QN, in0=ps(pb), scalar=qk2[:, gcol:gcol + 1], in1=RS, op0=ALU.mult, op1=ALU.mult),
                            r=[psr(pb), "qk2", "RS"], w=["QN"])
                        S.add('pe', lambda e: e.matmul(ps(5), lhsT=PERM, rhs=QN, start=True, stop=True),
                              r=["PERM", "QN"], w=[psr(5)])
                        S.add('pool', lambda e, cs=cs: e.tensor_tensor(out=T1, in0=QN, in1=ROPE[:, 0, cs], op=ALU.mult),
                              r=["QN", "ROPE"], w=["T1"])
                        S.add('dve', lambda e, cs=cs: e.tensor_tensor(out=T2, in0=ROPE[:, 1, cs], in1=ps(5), op=ALU.mult),
                              r=[psr(5), "ROPE"], w=["T2"])
                        if kind_ == "q":
                            S.add('dve', lambda e, gc=gc, cs=cs: e.tensor_tensor(out=QT[:, gc, cs], in0=T1, in1=T2, op=ALU.add),
                                  r=["T1", "T2"], w=[("QT", gc)])
                        else:
                            kst = KST[kst_i % 2]
                            kn = ("KST", kst_i % 2)
                            kst_i += 1
                            kc0 = 256 + th * 512
                            S.add('dve', lambda e, kst=kst: e.tensor_tensor(out=kst, in0=T1, in1=T2, op=ALU.add),
                                  r=["T1", "T2"], w=[kn])
                            S.add('sp', lambda e, kst=kst, gc=gc, cs=cs: e.dma_start(
                                out=kT_out[gc * 128:(gc + 1) * 128, cs], in_=kst), r=[kn], w=[("kout", gc, th)], dma=kn)
                            S.add('act', lambda e, kst=kst, gc=gc, kc0=kc0: e.copy(
                                out=KT[0:64, 2 * gc, kc0:kc0 + 512], in_=kst[0:64, :]), r=[kn], w=[("KT", 2 * gc, 0, th)])
                            S.add('act', lambda e, kst=kst, gc=gc, kc0=kc0: e.copy(
                                out=KT[64:128, 2 * gc + 1, kc0:kc0 + 512], in_=kst[64:128, :]), r=[kn],
                                w=[("KT", 2 * gc + 1, 1, th)])
                            S.add('pe', lambda e, kst=kst: e.matmul(ps(6), lhsT=SWP, rhs=kst, start=True, stop=True),
                                  r=["SWP", kn], w=[psr(6)])
                            S.add('act', lambda e, gc=gc, kc0=kc0: e.copy(
                                out=KT[64:128, 2 * gc, kc0:kc0 + 512], in_=ps(6)[64:128, :]), r=[psr(6)],
                                w=[("KT", 2 * gc, 1, th)])
                            S.add('act', lambda e, gc=gc, kc0=kc0: e.copy(
                                out=KT[0:64, 2 * gc + 1, kc0:kc0 + 512], in_=ps(6)[0:64, :]), r=[psr(6)],
                                w=[("KT", 2 * gc + 1, 0, th)])
            if vcols is not None:
                c0, ncol, vo = vcols
                for tt in range(8):
                    pb = tt % 2
                    ts_ = slice(tt * 128, (tt + 1) * 128)
                    for c in range(NC_):
                        S.add('pe', lambda e, wb=wb, c=c, ts_=ts_, pb=pb, ncol=ncol, c0=c0: e.matmul(
                            ps(pb)[:, 0:ncol], lhsT=hb[:, c, ts_], rhs=wb[:, c, c0:c0 + ncol],
                            start=(c == 0), stop=(c == NC_ - 1)), r=[nm, ("hb", c)], w=[psr(pb)])
                    vst = VST[tt % 2]
                    vn = ("VST", tt % 2)
                    h0 = vo // 64
                    nh = ncol // 64
                    S.add('act', lambda e, vst=vst, pb=pb, vo=vo, ncol=ncol: e.copy(out=vst[:, vo:vo + ncol], in_=ps(pb)[:, 0:ncol]),
                          r=[psr(pb)], w=[vn])
                    S.add('dve', lambda e, tt=tt, pb=pb, h0=h0, nh=nh, ncol=ncol: e.tensor_copy(
                        out=VA[:, 2 + tt, h0:h0 + nh, 0:64],
                        in_=VST[tt % 2][:, h0 * 64:h0 * 64 + ncol].rearrange("p (h d) -> p h d", d=64)),
                        r=[("VST", tt % 2)], w=[("VA", tt, vo)])
                    S.add('sp', lambda e, vst=vst, ts_=ts_, vo=vo, ncol=ncol: e.dma_start(
                        out=v_out[ts_, vo:vo + ncol], in_=vst[:, vo:vo + ncol]), r=[vn], w=[("vout", tt, vo)], dma=vn)
        A.release()
        A.mark()
        MASK = A.bf16([128, 10, T])
        E = A.bf16([128, 10, T])
        CB2 = [A.bf16([128, 16 * 64]) for _ in range(2)] if is_na else None
        sp_load(MASK, mask_in, "MASK")
        qt_all = [("QT", c) for c in range(NC_)]
        kt_all = ([("KT", c) for c in range(ng)] if is_na else
                  [("KT", g_, hf, th) for g_ in range(4) for hf in range(2) for th in range(2)]) + [("KTc",)]
        va_all = [("VA1",), ("VAc", 0), ("VAc", 1)] + [("VA", tt, vo) for tt in range(8) for vo in (range(0, NV, 512) if is_na else [0])]
        for h in range(16):
            hp = (h % 2) * 64
            qc = h // 2
            if is_na:
                ksl = KT[hp:hp + 64, h // 2, :]
                g = h
                cbn = ("CB2", h % 2)
                S.add('sp', lambda e, h=h: e.dma_start(out=CB2[h % 2], in_=cb2_in[h]), w=[cbn], dma=cbn)
            else:
                ksl = KT[hp:hp + 64, h // 4, :]
                g = h // 4
            for b in range(10):
                banks = (0, 1) if b % 2 == 0 else (2, 3)
                for th in range(2):
                    S.add('pe', lambda e, ksl=ksl, b=b, th=th, hp=hp, qc=qc, banks=banks: e.matmul(
                        ps(banks[th]), lhsT=ksl[:, b * 128:(b + 1) * 128],
                        rhs=QT[hp:hp + 64, qc, th * 512:(th + 1) * 512], start=True, stop=False),
                        r=qt_all + kt_all, w=[psr(banks[th])])
                if is_na and b >= 2:
                    bl = b - 2
                    qlo, qhi = max(0, 2 * bl - 7), min(15, 2 * bl + 8)
                    for th in range(2):
                        qa, qb = max(qlo, 8 * th), min(qhi, 8 * th + 7)
                        if qa > qb:
                            continue
                        ja, jb = qa - 2 * bl + 7, qb - 2 * bl + 7
                        S.add('pe', lambda e, th=th, qa=qa, qb=qb, ja=ja, jb=jb, banks=banks, h=h: e.matmul(
                            ps(banks[th])[:, (qa - 8 * th) * 64:(qb + 1 - 8 * th) * 64], lhsT=ident_b,
                            rhs=CB2[h % 2][:, ja * 64:(jb + 1) * 64], start=False, stop=False),
                            r=[cbn, "ident_b"], w=[psr(banks[th])])
                for th in range(2):
                    S.add('pe', lambda e, b=b, th=th, banks=banks: e.matmul(
                        ps(banks[th]), lhsT=ident_b, rhs=MASK[:, b, th * 512:(th + 1) * 512], start=False, stop=True),
                        r=["MASK", "ident_b"], w=[psr(banks[th])])
                for th in range(2):
                    S.add('act', lambda e, b=b, th=th, banks=banks: e.activation(
                        out=E[:, b, th * 512:(th + 1) * 512], in_=ps(banks[th]), func=AF.Exp),
                        r=[psr(banks[th])], w=[("E", b)])
            ob = 4 + 2 * (h % 2)
            for qt in range(8):
                bank = ob + qt // 4
                off = (qt % 4) * 128
                for b in range(10):
                    S.add('pe', lambda e, qt=qt, b=b, bank=bank, off=off, g=g: e.matmul(
                        ps(bank)[:, off:off + 65], lhsT=E[:, b, qt * 128:(qt + 1) * 128], rhs=VA[:, b, g, :],
                        start=(b == 0), stop=(b == 9)), r=[("E", b)] + va_all, w=[psr(bank)])
            for half in range(2):
                bank = ob + half
                S.add('dve', lambda e, bank=bank, half=half: e.reciprocal(
                    out=REC[:, half * 4:(half + 1) * 4],
                    in_=ps(bank).rearrange("p (q c) -> p q c", c=128)[:, :, 64]), r=[psr(bank)], w=[("REC", half)])
                for q4 in range(4):
                    qt = half * 4 + q4
                    S.add('dve', lambda e, bank=bank, q4=q4, qt=qt, h=h: e.tensor_scalar_mul(
                        out=OTK[:, qt, h * 64:(h + 1) * 64], in0=ps(bank)[:, q4 * 128:q4 * 128 + 64],
                        scalar1=REC[:, qt:qt + 1]), r=[psr(bank), ("REC", half)], w=[("OTK", qt)])
        A.release()
        A.mark()
        Wo = A.bf16([128, NC_, 1024])
        S.add('pool', lambda e: e.dma_start(out=Wo, in_=wo_d[j].rearrange("(c p) f -> p c f", p=128)), w=["Wo"], dma="Wo")
        for fc in range(NC_):
            for qg in range(2):
                pb = (fc * 2 + qg) % 4
                pst = PS[pb][:, 0:256].bitcast(BF16)
                for k in range(4):
                    S.add('pe', lambda e, pst=pst, k=k, qg=qg, fc=fc: e.transpose(
                        out=pst[:, k * 128:(k + 1) * 128], in_=OTK[:, qg * 4 + k, fc * 128:(fc + 1) * 128],
                        identity=ident_b), r=[("OTK", qg * 4 + k), "ident_b"], w=[psr(pb)])
                S.add('act', lambda e, pst=pst, fc=fc, qg=qg: e.copy(out=hb[:, fc, qg * 512:(qg + 1) * 512], in_=pst),
                      r=[psr(pb)], w=[("hb", fc)])
        for fc in range(NC_):
            for th in range(2):
                cs = slice(th * 512, (th + 1) * 512)
                pb = (fc * 2 + th) % 4
                for c in range(NC_):
                    S.add('pe', lambda e, fc=fc, c=c, cs=cs, pb=pb: e.matmul(
                        ps(pb), lhsT=Wo[:, c, fc * 128:(fc + 1) * 128], rhs=hb[:, c, cs],
                        start=(c == 0), stop=(c == NC_ - 1)), r=["Wo", ("hb", c)], w=[psr(pb)])
                residual_from_psum(fc, th, pb, 16)
        A.release()
        A.release()

    def moe(l, last):
        A.mark()
        o_w1 = [A.alloc(NC_ * 2048 // 2) for _ in range(2)]
        W1 = [A.bf16_at(o, [128, NC_, 2048]) for o in o_w1]
        h32 = A.f32_at(o_w1[1], [128, NC_, T])
        W2 = A.bf16([128, NC_, 1024])
        aT = A.bf16([128, NC_, T])
        GS = RS
        US = A.f32([128, 512])
        SIG = SQ
        CB = A.f32([128, T])
        b1 = A.f32([128, NE, 16])
        b1s = A.f32([128, NE, 8])
        rw = A.f32([128, NC_, NE])
        rb = A.f32([1, NE])
        o_cb = A.alloc(T)
        combT = A.f32_at(o_cb, [32, T])
        cm = A.f32([32, T])
        b2 = cm
        lg = A.f32([128, NE])
        mx8 = A.f32([128, 8])
        ex = A.f32([128, NE])
        sm = A.f32([128, 4])
        cst = A.f32([128, 4])
        for ci_, val in enumerate((7.0, 8.0, S7, -6.0)):
            S.add('dve', lambda e, ci_=ci_, val=val: e.memset(cst[:, ci_:ci_ + 1], val), w=[("cst", ci_)])
        cstr = [("cst", q) for q in range(4)]

        sp_load(b1, b1_in[:, l], "b1")
        sp_load(b2, moe_b2[l], "cm")
        sp_load(rw, router_w[l].rearrange("(c p) e -> p c e", p=128), "rw")
        sp_load(rb, router_b[l:l + 1, :], "rb")
        S.add('dve', lambda e: e.tensor_scalar_mul(out=b1s, in0=b1[:, :, 0:8], scalar1=1.702), r=["b1"], w=["b1s"])
        S.add('dve', lambda e: e.tensor_scalar_add(out=b1[:, :, 8:16], in0=b1[:, :, 8:16], scalar1=1.0),
              r=["b1"], w=["b1"])

        layernorm(l, 1, 24, h32=h32)

        for tt in range(8):
            ts_ = slice(tt * 128, (tt + 1) * 128)
            for c in range(NC_):
                S.add('pe', lambda e, c=c, ts_=ts_: e.matmul(ps(4)[:, 0:NE], lhsT=h32[:, c, ts_], rhs=rw[:, c, :],
                                                              start=(c == 0), stop=False),
                      r=[("h32", c), "rw"], w=[psr(4)])
            S.add('pe', lambda e: e.matmul(ps(4)[:, 0:NE], lhsT=ones_f[0:1, 0:128], rhs=rb, start=False, stop=True),
                  r=["ones_f", "rb"], w=[psr(4)])
            S.add('dve', lambda e: e.tensor_copy(out=lg, in_=ps(4)[:, 0:NE]), r=[psr(4)], w=["lg"])
            S.add('dve', lambda e: e.max(out=mx8, in_=lg), r=["lg"], w=["mx8"])
            S.add('dve', lambda e: e.tensor_scalar_mul(out=sm[:, 0:1], in0=mx8[:, 0:1], scalar1=-1.0),
                  r=["mx8"], w=["sm0"])
            S.add('act', lambda e: e.activation(out=ex, in_=lg, func=AF.Exp, bias=sm[:, 0:1], scale=1.0),
                  r=["lg", "sm0"], w=["ex"])
            S.add('dve', lambda e: e.scalar_tensor_tensor(out=ex, in0=lg, scalar=mx8[:, 3:4], in1=ex,
                                                          op0=ALU.is_ge, op1=ALU.mult),
                  r=["lg", "mx8", "ex"], w=["ex"])
            S.add('dve', lambda e: e.reduce_sum(out=sm[:, 1:2], in_=ex, axis=mybir.AxisListType.X),
                  r=["ex"], w=["sm1"])
            S.add('dve', lambda e: e.reciprocal(out=sm[:, 2:3], in_=sm[:, 1:2]), r=["sm1"], w=["sm2"])
            S.add('dve', lambda e: e.tensor_scalar_mul(out=ex, in0=ex, scalar1=sm[:, 2:3]), r=["ex", "sm2"], w=["ex"])
            S.add('pe', lambda e: e.transpose(out=ps(4)[0:32, 128:256], in_=ex, identity=ident_f),
                  r=["ex", "ident_f"], w=[psr(4)])
            S.add('dve', lambda e, ts_=ts_: e.tensor_copy(out=combT[:, ts_], in_=ps(4)[0:32, 128:256]),
                  r=[psr(4)], w=["combT"])

        for c in range(NC_):
            for th in range(2):
                cs = slice(th * 512, (th + 1) * 512)
                pb = 4 + (c * 2 + th) % 2
                S.add('pe', lambda e, c=c, cs=cs, pb=pb: e.matmul(ps(pb), lhsT=b2[:, c * 128:(c + 1) * 128],
                                                                   rhs=combT[:, cs], start=True, stop=True),
                      r=["cm", "combT"], w=[psr(pb)])
                S.add('act', lambda e, c=c, cs=cs, pb=pb: e.copy(out=R[:, c, cs], in_=ps(pb)),
                      r=[psr(pb)], w=[("R", c, th)])

        def load_w1(e_):
            nm = ("W1", e_ % 2)
            S.add('pool', lambda e: e.dma_start(out=W1[e_ % 2],
                                                in_=moe_w1[l, e_].rearrange("(c p) f -> p c f", p=128)),
                  w=[nm] + ([("h32", c) for c in range(NC_)] if e_ % 2 == 1 else []), dma=nm)

        def load_w2(e_):
            S.add('pool', lambda e: e.dma_start(out=W2, in_=moe_w2[l, e_].rearrange("(c p) f -> p c f", p=128)),
                  w=["W2"], dma="W2")

        def cm_for(e_):
            S.add('dve', lambda e: e.tensor_scalar_mul(out=cm, in0=combT, scalar1=ident_f[0:32, e_:e_ + 1]),
                  r=["combT", "ident_f"], w=["cm"])

        def cb_half(e_, th):
            cs = slice(th * 512, (th + 1) * 512)
            S.add('pe', lambda e: e.matmul(ps(6 + th), lhsT=ones_f[0:32, 0:128], rhs=cm[:, cs], start=True, stop=True),
                  r=["ones_f", "cm"], w=[psr(6 + th)])
            S.add('act', lambda e: e.copy(out=CB[:, cs], in_=ps(6 + th)), r=[psr(6 + th)], w=[("CB", th)])

        def h1_step(e_, i, th):
            if SUB < 2:
                return
            cs = slice(th * 512, (th + 1) * 512)
            w1 = W1[e_ % 2]
            nm = ("W1", e_ % 2)
            sb = (i + th) % 2
            pg, pu = 2 * sb, 2 * sb + 1
            for c in range(NC_):
                S.add('pe', lambda e, c=c: e.matmul(ps(pg), lhsT=w1[:, c, i * 128:(i + 1) * 128], rhs=hb[:, c, cs],
                                                    start=(c == 0), stop=(c == NC_ - 1)),
                      r=[nm, ("hb", c)], w=[psr(pg)])
            for c in range(NC_):
                S.add('pe', lambda e, c=c: e.matmul(ps(pu), lhsT=w1[:, c, 1024 + i * 128:1024 + (i + 1) * 128],
                                                    rhs=hb[:, c, cs], start=(c == 0), stop=(c == NC_ - 1)),
                      r=[nm, ("hb", c)], w=[psr(pu)])
            sg = SIG[sb]
            if SUB < 3:
                return
            S.add('act', lambda e: e.activation(out=GS, in_=ps(pg), func=AF.Identity, bias=b1[:, e_, i:i + 1], scale=1.0),
                  r=[psr(pg), "b1"], w=["RS"])
            S.add('act', lambda e: e.activation(out=sg, in_=ps(pg), func=AF.Sigmoid, bias=b1s[:, e_, i:i + 1], scale=1.702),
                  r=[psr(pg), "b1s"], w=[("SQ", sb)])
            S.add('act', lambda e: e.activation(out=US, in_=ps(pu), func=AF.Identity, bias=b1[:, e_, 8 + i:9 + i], scale=1.0),
                  r=[psr(pu), "b1"], w=["US"])
            if SUB < 4:
                return
            S.add('dve', lambda e: e.tensor_scalar_min(out=GS, in0=GS, scalar1=7.0), r=["RS"], w=["RS"])
            S.add('dve', lambda e: e.tensor_scalar(out=US, in0=US, scalar1=8.0, scalar2=-6.0, op0=ALU.min, op1=ALU.max),
                  r=["US"], w=["US"])
            S.add('dve', lambda e: e.scalar_tensor_tensor(out=GS, in0=sg, scalar=S7, in1=GS, op0=ALU.min, op1=ALU.mult),
                  r=["RS", ("SQ", sb)], w=["RS"])
            S.add('dve', lambda e: e.tensor_tensor(out=US, in0=US, in1=GS, op=ALU.mult), r=["US", "RS"], w=["US"])
            S.add('dve', lambda e: e.tensor_tensor(out=aT[:, i, cs], in0=US, in1=CB[:, cs], op=ALU.mult),
                  r=["US", ("CB", th)], w=[("aT", i, th)])

        def y_step(e_, c, th):
            if SUB < 5:
                return
            cs = slice(th * 512, (th + 1) * 512)
            pb = 4 + (c + th) % 2
            for k in range(NC_):
                S.add('pe', lambda e, k=k: e.matmul(ps(pb), lhsT=W2[:, k, c * 128:(c + 1) * 128], rhs=aT[:, k, cs],
                                                    start=(k == 0), stop=(k == NC_ - 1)),
                      r=["W2", ("aT", k, th)], w=[psr(pb)])
            S.add('dve', lambda e: e.tensor_tensor(out=R[:, c, cs], in0=R[:, c, cs], in1=ps(pb), op=ALU.add),
                  r=[psr(pb), ("R", c, th)], w=[("R", c, th)])

        NEX = min(NE, NED) if STAGE >= 4 else 0
        if NEX > 0:
            load_w1(0)
        if NEX > 1:
            load_w1(1)
        PULL = 2
        for e_ in range(NEX):
            if e_ == 0:
                load_w2(0)
                cm_for(0)
                cb_half(0, 0)
                cb_half(0, 1)
                for i in range(NC_):
                    h1_step(0, i, 0)
            if e_ + 1 < NEX:
                cm_for(e_ + 1)
            for i in range(NC_):
                h1_step(e_, i, 1)
                y_step(e_, i, 0)
            if e_ + 2 < NEX:
                load_w1(e_ + 2)
            if e_ + 1 < NEX:
                cb_half(e_ + 1, 0)
                for i in range(PULL):
                    h1_step(e_ + 1, i, 0)
            for c in range(NC_):
                y_step(e_, c, 1)
            if e_ + 1 < NEX:
                cb_half(e_ + 1, 1)
                load_w2(e_ + 1)
                for i in range(PULL, NC_):
                    h1_step(e_ + 1, i, 0)

        for c in range(NC_):
            for th in range(2):
                cs = slice(th * 512, (th + 1) * 512)
                S.add('dve', lambda e, c=c, cs=cs: e.scalar_tensor_tensor(
                    out=R[:, c, cs], in0=R[:, c, cs], scalar=modT[:, 40 + c:41 + c], in1=xT[:, c, cs],
                    op0=ALU.mult, op1=ALU.add), r=["modT", ("xT", c), ("R", c, th)], w=[("R", c, th)])
        A.release()

    for li, (l, kind, j) in enumerate(layers):
        if STAGE >= 1:
            adaln(l)
            modulate(xT, 0)
        if STAGE >= 2:
            if kind == 0:
                conv_mixer(l, j)
            elif kind == 1:
                attention(l, j, False)
            else:
                attention(l, j, True)
        if STAGE >= 3:
            moe(l, li == len(layers) - 1)
        if STAGE >= 5:
            layernorm(l, 2, None)

    for c in range(NC_):
        nm = ("yst", c)
        i = S.add('sp', lambda e, c=c: e.dma_start(out=yT_out[c * 128:(c + 1) * 128, :], in_=xT[:, c, :]),
                  r=[("xT", c)] + [("xTh", c, th) for th in range(2)], w=[nm], dma=nm)
        final_waits.append(i)

    S.emit(nc, final_waits)
    es.close()
    return nc


def _fm(v):
    v = np.asarray(v, np.float32)
    lead = v.shape[:-1]
    return np.ascontiguousarray(np.moveaxis(v.reshape(lead + (NC_, 128)), -1, 0))


def _prep_common(inp):
    L = DEPTH
    com = {}
    com["w_mod"] = np.ascontiguousarray(inp["w_mod"][:LD], np.float32)
    com["bmod"] = np.ascontiguousarray(
        np.asarray(inp["b_mod"], np.float32).reshape(L, 48, 128).transpose(2, 0, 1))
    lnp = np.stack([inp["ln1_g"], inp["ln1_b"], inp["ln2_g"], inp["ln2_b"]], axis=1)
    com["lnp"] = np.ascontiguousarray(np.asarray(lnp, np.float32).reshape(L, 4, NC_, 128).transpose(3, 0, 1, 2))
    com["router_w"] = np.ascontiguousarray(inp["router_w"], np.float32)
    com["router_b"] = np.ascontiguousarray(inp["router_b"], np.float32)
    com["moe_w1"] = np.ascontiguousarray(inp["moe_w1"][:LD, :NED], np.float32)
    com["moe_w2"] = np.ascontiguousarray(inp["moe_w2"][:LD, :NED], np.float32)
    com["b1"] = np.ascontiguousarray(
        np.asarray(inp["moe_b1"], np.float32).reshape(L, NE, 16, 128).transpose(3, 0, 1, 2))
    com["moe_b2"] = np.ascontiguousarray(inp["moe_b2"], np.float32)
    com["conv_w_in"] = np.ascontiguousarray(inp["conv_w_in"], np.float32)
    com["conv_w_out"] = np.ascontiguousarray(inp["conv_w_out"], np.float32)
    cw = np.asarray(inp["conv_w"], np.float32)
    cbias = np.asarray(inp["conv_b"], np.float32)
    cp = np.concatenate([cw, cbias[:, None, :]], axis=1)
    com["convp"] = np.ascontiguousarray(cp.reshape(2, 4, NC_, 128).transpose(3, 0, 1, 2))
    com["ident_f"] = np.eye(128, dtype=np.float32)
    com["ident_b"] = np.eye(128, dtype=np.float32).astype(ml_dtypes.bfloat16)
    com["onesD"] = np.full((128, 128), 1.0 / D, np.float32)
    com["ones_f"] = np.ones((128, 128), np.float32)
    com["attn_w_qkv"] = np.ascontiguousarray(inp["attn_w_qkv"], np.float32)
    com["attn_w_o"] = np.ascontiguousarray(inp["attn_w_o"], np.float32)
    qn = np.asarray(inp["attn_q_norm"], np.float32)[0]
    kn = np.asarray(inp["attn_k_norm"], np.float32)[0]
    com["qkn"] = np.ascontiguousarray(np.stack([np.tile(qn, 2), np.tile(kn, 2)], axis=1))
    com["na_w_qkv"] = np.ascontiguousarray(inp["na_w_qkv"], np.float32)
    com["na_w_o"] = np.ascontiguousarray(inp["na_w_o"], np.float32)
    P = np.zeros((128, 128), np.float32)
    for hh in range(2):
        for half in range(2):
            b = hh * 64 + half * 32
            for i in range(16):
                P[b + 16 + i, b + i] = -1.0
                P[b + i, b + 16 + i] = 1.0
    com["perm"] = P
    blk = np.zeros((128, 128), np.float32)
    blk[:64, :64] = 1.0 / 64
    blk[64:, 64:] = 1.0 / 64
    com["blk"] = blk
    sw = np.zeros((128, 128), np.float32)
    for m in range(128):
        sw[(m + 64) % 128, m] = 1.0
    com["swp"] = sw
    return com


def _prep_core(inp, ci):
    d = {}
    if ci < 2:
        tok = np.asarray(inp["x_sample"], np.float32)[ci]
        cond = np.asarray(inp["c"], np.float32)[ci]
        seqlen = 1024
    else:
        s0 = 4 * (ci - 2)
        tok = np.asarray(inp["x_prompt"], np.float32)[s0:s0 + 4].reshape(T, D)
        cond = np.asarray(inp["c_ctx"], np.float32)
        seqlen = 256
    d["xT_in"] = np.ascontiguousarray(tok.T)
    d["cond"] = np.ascontiguousarray(cond.reshape(NC_, 128).T)
    t = np.arange(T)
    ml = (t % seqlen != 0).astype(np.float32)
    mr = (t % seqlen != seqlen - 1).astype(np.float32)
    d["cmask"] = np.ascontiguousarray(np.broadcast_to(np.stack([ml, mr])[None], (128, 2, T)))
    bf = ml_dtypes.bfloat16
    k = np.arange(1280)
    q = np.arange(T)
    tk = k - 256
    if ci < 2:
        inv = (np.float32(10000.0) ** (-np.arange(16, dtype=np.float32) / np.float32(16))).astype(np.float32)
        row = (t // 64).astype(np.float32)
        col = (t % 64).astype(np.float32)
        cos = np.zeros((128, T), np.float32)
        sin = np.zeros((128, T), np.float32)
        for p in range(128):
            dd = p % 64
            pos = row if dd < 32 else col
            ang = (pos * inv[(dd % 32) % 16]).astype(np.float32)
            cos[p] = np.cos(ang)
            sin[p] = np.sin(ang)
        d["rope"] = np.ascontiguousarray(np.stack([cos, sin], axis=1))
        ck = np.asarray(inp["cache_k_attn"], np.float32)[ci, 0]
        d["ckT_attn"] = np.ascontiguousarray(np.tile(ck.transpose(2, 1, 0), (2, 1, 1)))
        d["cv_attn"] = np.ascontiguousarray(np.asarray(inp["cache_v_attn"], np.float32)[ci, 0].reshape(256, 256))
        d["ckT_na"] = np.ascontiguousarray(np.asarray(inp["cache_k_na"], np.float32)[ci, 0].reshape(256, D).T)
        d["cv_na"] = np.ascontiguousarray(np.asarray(inp["cache_v_na"], np.float32)[ci, 0].reshape(256, D))
        am = np.zeros((1280, T), np.float32)
        kr, kc = tk // 64, tk % 64
        qr, qc = q // 64, q % 64
        rs = np.clip(qr - 4, 0, 8)
        cs = np.clip(qc - 8, 0, 48)
        ok = ((kr[:, None] >= rs[None]) & (kr[:, None] < rs[None] + 8) &
              (kc[:, None] >= cs[None]) & (kc[:, None] < cs[None] + 16))
        nm = np.where(ok, 0.0, NEG).astype(np.float32)
        nm[:256] = 0.0
        rpb = np.asarray(inp["na_rpb"], np.float32)[0]
        cb2 = np.zeros((16, 2, 64, 16, 64), np.float32)
        kcs = np.arange(64)
        co = kcs[:, None] - kcs[None, :] + 15
        cok = (co >= 0) & (co <= 30)
        coc = np.clip(co, 0, 30)
        for dr in range(2):
            for jj in range(16):
                ro = 14 - jj + dr
                if 0 <= ro <= 14:
                    cb2[:, dr, :, jj, :] = np.where(cok[None], rpb[:, ro][:, coc], 0.0)
        d["cb2"] = np.ascontiguousarray(cb2.reshape(16, 128, 1024)).astype(bf)
    else:
        d["rope"] = np.ascontiguousarray(np.stack([np.ones((128, T), np.float32), np.zeros((128, T), np.float32)], axis=1))
        d["ckT_attn"] = np.zeros((128, 4, 256), np.float32)
        d["cv_attn"] = np.zeros((256, 256), np.float32)
        d["ckT_na"] = np.zeros((D, 256), np.float32)
        d["cv_na"] = np.zeros((256, D), np.float32)
        ok = (tk[:, None] // 256) == (q[None] // 256)
        am = np.where(ok, 0.0, NEG).astype(np.float32)
        am[:256] = NEG
        nm = am
        d["cb2"] = np.zeros((16, 128, 1024), bf)
    d["amask"] = np.ascontiguousarray(am.reshape(10, 128, T).transpose(1, 0, 2)).astype(bf)
    d["nmask"] = np.ascontiguousarray(nm.reshape(10, 128, T).transpose(1, 0, 2)).astype(bf)
    return d


_PROG_CACHE = {}


def _get_prog(layers):
    key = tuple(layers)
    if key not in _PROG_CACHE:
        _PROG_CACHE[key] = build_program(list(layers))
    return _PROG_CACHE[key]


def kernel(**inputs):
    layers = [(l, l % 3, l // 3) for l in range(DEPTH)]
    nc = _get_prog(layers)
    com = _prep_common(inputs)
    in_maps = []
    for ci in range(8):
        d = dict(com)
        d.update(_prep_core(inputs, min(ci, NCORES - 1)))
        in_maps.append(d)
    res = run_bass_kernel_spmd(nc, in_maps, core_ids=list(range(8)))
    R_ = res.results
    y_s = np.stack([np.asarray(R_[ci]["yT_out"]).T for ci in range(2)]).astype(np.float32)
    y_p = np.concatenate([np.asarray(R_[ci]["yT_out"]).T.reshape(4, 256, D) for ci in range(2, 6)]).astype(np.float32)
    nk_a = np.concatenate([np.asarray(R_[ci]["kT_attn_out"]).T.reshape(4, 256, 4, 64) for ci in range(2, 6)])
    nv_a = np.concatenate([np.asarray(R_[ci]["v_attn_out"]).reshape(4, 256, 4, 64) for ci in range(2, 6)])
    nk_n = np.concatenate([np.asarray(R_[ci]["kT_na_out"]).T.reshape(4, 256, 16, 64) for ci in range(2, 6)])
    nv_n = np.concatenate([np.asarray(R_[ci]["v_na_out"]).reshape(4, 256, 16, 64) for ci in range(2, 6)])
    f = lambda a: np.ascontiguousarray(a[:, None], dtype=np.float32)
    return (np.ascontiguousarray(y_p), np.ascontiguousarray(y_s), f(nk_a), f(nv_a), f(nk_n), f(nv_n))
```
